# Optimizing a Trainium2 kernel written in Bass

```python
import jax, jax.numpy as jnp
from jax import lax
import numpy as np

D_MODEL = 1024
BATCH = 4
SEQ = 4096
DEPTH = 2

GRID_W = 64
CTX_LEN = 256
D_MIX = D_MODEL
RMS_EPS = 1e-6
A_WIDTH = D_MIX // 4
A_HEADS = 4
A_HD = A_WIDTH // A_HEADS
A_CONV = 4
LRU_C = 8.0
A_COLS = 2 * A_WIDTH
B_WIDTH = D_MIX // 4
B_HD = 64
B_HEADS = B_WIDTH // B_HD
DECAY_RANK = 64
ICLR_RANK = 64
GATE_RANK = 64
GN_EPS = 64e-5
B_COLS = 3 * B_WIDTH + DECAY_RANK + ICLR_RANK + GATE_RANK
B_SPLITS = (B_WIDTH, 2 * B_WIDTH, 3 * B_WIDTH, 3 * B_WIDTH + DECAY_RANK,
            3 * B_WIDTH + DECAY_RANK + ICLR_RANK)
C_WIDTH = D_MIX // 2
C_HD = 64
C_HEADS = C_WIDTH // C_HD
WIN_H = 8
WIN_W = 16
C_COLS = 3 * C_WIDTH
IN_COLS = A_COLS + B_COLS + C_COLS
N_EXPERTS = 32
TOP_K = 4
D_FF = D_MODEL
SWIGLU_ALPHA = 1.702
SWIGLU_LIMIT = 7.0
MOE_BLOCK = 128

kernel_name = 'hybrid_lru_rwkv7_natten_moe_dit'


def _rmsnorm(x, g):
    xf = x.astype(jnp.float32)
    y = xf * lax.rsqrt(jnp.mean(xf * xf, axis=-1, keepdims=True) + RMS_EPS)
    return (y * g.astype(jnp.float32)).astype(x.dtype)


def _modulate(h, shift, scale):
    return h * (1 + scale) + shift


def _dir_conv(x, w, b, reverse):
    t = x.shape[1]
    kw = w.shape[0]
    if reverse:
        xp = jnp.pad(x, ((0, 0), (0, kw - 1), (0, 0)))
        taps = [xp[:, j:j + t] for j in range(kw)]
    else:
        xp = jnp.pad(x, ((0, 0), (kw - 1, 0), (0, 0)))
        taps = [xp[:, kw - 1 - j:kw - 1 - j + t] for j in range(kw)]
    return sum(w[j] * taps[j] for j in range(kw)) + b


def _lin_scan(a, b, h0, reverse):
    def combine(e1, e2):
        a1, b1 = e1
        a2, b2 = e2
        return a1 * a2, a2 * b1 + b2
    a_cum, h = lax.associative_scan(combine, (a, b), reverse=reverse, axis=1)
    return h + a_cum * h0[:, None, :]


def _rglru(xc, wr, br, wi, bi, lam, h0, reverse):
    bsz, t, _ = xc.shape
    xf = xc.astype(jnp.float32)
    xh = xf.reshape(bsz, t, A_HEADS, A_HD)
    gate_r = jax.nn.sigmoid(jnp.einsum('bthi,hij->bthj', xh, wr.astype(jnp.float32)).reshape(bsz, t, A_WIDTH) + br)
    gate_i = jax.nn.sigmoid(jnp.einsum('bthi,hij->bthj', xh, wi.astype(jnp.float32)).reshape(bsz, t, A_WIDTH) + bi)
    log_a = -LRU_C * gate_r * jax.nn.softplus(-lam.astype(jnp.float32))
    a = jnp.exp(log_a)
    b = jnp.sqrt(-jnp.expm1(2.0 * log_a)) * (gate_i * xf)
    return _lin_scan(a, b, h0, reverse)


def _lru_mixer(pc, pl, conv_w, conv_b, wr, br, wi, bi, lam, need_ctx_out):
    xc, gc = pc[..., :A_WIDTH], pc[..., A_WIDTH:]
    xl, gl = pl[..., :A_WIDTH], pl[..., A_WIDTH:]
    h0 = jnp.zeros((pc.shape[0], A_WIDTH), jnp.float32)
    hc_dirs, hl_dirs = [], []
    for d, rev in enumerate((False, True)):
        hc = _rglru(_dir_conv(xc, conv_w[d], conv_b[d], rev), wr[d], br[d], wi[d], bi[d], lam[d], h0, rev)
        h_end = hc[:, 0] if rev else hc[:, -1]
        hl = _rglru(_dir_conv(xl, conv_w[d], conv_b[d], rev), wr[d], br[d], wi[d], bi[d], lam[d], h_end, rev)
        hc_dirs.append(hc)
        hl_dirs.append(hl)
    out_l = (jax.nn.gelu(gl.astype(jnp.float32)) * (hl_dirs[0] + hl_dirs[1])).astype(pl.dtype)
    out_c = None
    if need_ctx_out:
        out_c = (jax.nn.gelu(gc.astype(jnp.float32)) * (hc_dirs[0] + hc_dirs[1])).astype(pc.dtype)
    return out_c, out_l


def _token_shift(p, mu):
    prev = jnp.pad(p[:, :-1], ((0, 0), (1, 0), (0, 0)))
    nxt = jnp.pad(p[:, 1:], ((0, 0), (0, 1), (0, 0)))
    return p + mu[0] * (prev - p) + mu[1] * (nxt - p)


def _heads(z):
    return z.reshape(z.shape[0], z.shape[1], B_HEADS, B_HD)


def _wkv7_scan(r, w, k, v, a_vec, b_vec, s0, reverse):
    def step(s, inp):
        r_t, w_t, k_t, v_t, a_t, b_t = inp
        sa = jnp.einsum('bhij,bhj->bhi', s, a_t)
        s = s * w_t[:, :, None, :] + sa[..., None] * b_t[:, :, None, :] + v_t[..., None] * k_t[:, :, None, :]
        return s, jnp.einsum('bhij,bhj->bhi', s, r_t)
    xs = tuple(jnp.moveaxis(z, 1, 0) for z in (r, w, k, v, a_vec, b_vec))
    s_end, ys = lax.scan(step, s0, xs, reverse=reverse)
    return jnp.moveaxis(ys, 0, 1), s_end


def _rwkv_dir(r, k, v, wlo, alo, w0, w2, a0, a2, k_k, k_a, s0, reverse):
    w = -jax.nn.softplus(-(w0 + jnp.tanh(wlo) @ w2)) - 0.5
    decay = jnp.exp(-jnp.exp(w))
    a = jax.nn.sigmoid(a0 + alo @ a2)
    kk = _heads(k * k_k)
    kk = kk / jnp.maximum(jnp.linalg.norm(kk, axis=-1, keepdims=True), 1e-12)
    k_eff = _heads(k * (1 + (a - 1) * k_a))
    y, s_end = _wkv7_scan(_heads(r), _heads(decay), k_eff, _heads(v), -kk, kk * _heads(a), s0, reverse)
    return y, s_end, k_eff


def _rwkv_finish(parts, outs, g2, r_k, lnx_g, lnx_b):
    r, v, glo = _heads(parts[0]), _heads(parts[2]), parts[5]
    y = outs[0][0] + outs[1][0]
    mean = jnp.mean(y, axis=-1, keepdims=True)
    var = jnp.mean(jnp.square(y - mean), axis=-1, keepdims=True)
    yn = (y - mean) * lax.rsqrt(var + GN_EPS)
    bonus = sum(jnp.sum(r * o[1] * r_k, axis=-1, keepdims=True) for o in outs)
    bsz, t = y.shape[:2]
    yn = yn.reshape(bsz, t, B_WIDTH) * lnx_g + lnx_b + (bonus * v).reshape(bsz, t, B_WIDTH)
    g = jax.nn.sigmoid(glo) @ g2
    return yn * g


def _rwkv_mixer(pc, pl, mu, w0, w2, a0, a2, g2, k_k, k_a, r_k, lnx_g, lnx_b, need_ctx_out):
    parts_c = jnp.split(_token_shift(pc, mu).astype(jnp.float32), B_SPLITS, axis=-1)
    parts_l = jnp.split(_token_shift(pl, mu).astype(jnp.float32), B_SPLITS, axis=-1)
    s0 = jnp.zeros((pc.shape[0], B_HEADS, B_HD, B_HD), jnp.float32)
    outs_c, outs_l = [], []
    for d, rev in enumerate((False, True)):
        yc, sc_end, kc_eff = _rwkv_dir(*parts_c[:5], w0[d], w2[d], a0[d], a2[d], k_k, k_a, s0, rev)
        yl, _, kl_eff = _rwkv_dir(*parts_l[:5], w0[d], w2[d], a0[d], a2[d], k_k, k_a, sc_end, rev)
        outs_c.append((yc, kc_eff))
        outs_l.append((yl, kl_eff))
    out_l = _rwkv_finish(parts_l, outs_l, g2, r_k, lnx_g, lnx_b).astype(pl.dtype)
    out_c = None
    if need_ctx_out:
        out_c = _rwkv_finish(parts_c, outs_c, g2, r_k, lnx_g, lnx_b).astype(pc.dtype)
    return out_c, out_l


def _na_mixer(pc, pl, rpb, need_ctx_out):
    bsz, ctx_len, _ = pc.shape
    seq = pl.shape[1]
    scale = C_HD ** -0.5
    qc, kc, vc = (z.reshape(bsz, ctx_len, C_HEADS, C_HD) for z in jnp.split(pc, 3, axis=-1))
    out_c = None
    if need_ctx_out:
        s = jnp.einsum('bqhd,bkhd->bhqk', qc * scale, kc).astype(jnp.float32)
        p = jax.nn.softmax(s, axis=-1).astype(vc.dtype)
        out_c = jnp.einsum('bhqk,bkhd->bqhd', p, vc).reshape(bsz, ctx_len, C_WIDTH)
    rows = seq // GRID_W
    kh = min(WIN_H, rows)
    n_win = kh * WIN_W
    qg, kg, vg = (z.reshape(bsz, rows, GRID_W, C_HEADS, C_HD) for z in jnp.split(pl, 3, axis=-1))
    col = jnp.arange(GRID_W)
    col_start = jnp.clip(col - WIN_W // 2, 0, GRID_W - WIN_W)
    cols = col_start[:, None] + jnp.arange(WIN_W)[None, :]
    col_bidx = cols - col[:, None] + (WIN_W - 1)

    def row_block(args):
        r, q_row = args
        rs = jnp.clip(r - kh // 2, 0, rows - kh)
        k_band = lax.dynamic_slice_in_dim(kg, rs, kh, axis=1)
        v_band = lax.dynamic_slice_in_dim(vg, rs, kh, axis=1)
        k_win = k_band[:, :, cols]
        v_win = v_band[:, :, cols]
        row_bidx = rs + jnp.arange(kh) - r + (WIN_H - 1)
        bias = rpb[:, row_bidx[:, None, None], col_bidx[None, :, :]]
        s_win = jnp.einsum('bqhd,biqjhd->bhqij', q_row * scale, k_win).astype(jnp.float32)
        s_win = s_win + jnp.transpose(bias, (0, 2, 1, 3))[None].astype(jnp.float32)
        s_ctx = jnp.einsum('bqhd,bkhd->bhqk', q_row * scale, kc).astype(jnp.float32)
        s = jnp.concatenate([s_win.reshape(bsz, C_HEADS, GRID_W, n_win), s_ctx], axis=-1)
        p = jax.nn.softmax(s, axis=-1).astype(vc.dtype)
        p_win = p[..., :n_win].reshape(bsz, C_HEADS, GRID_W, kh, WIN_W)
        p_ctx = p[..., n_win:]
        return (jnp.einsum('bhqij,biqjhd->bqhd', p_win, v_win)
                + jnp.einsum('bhqk,bkhd->bqhd', p_ctx, vc))

    out = lax.map(row_block, (jnp.arange(rows), jnp.moveaxis(qg, 1, 0)))
    out_l = jnp.moveaxis(out, 0, 1).reshape(bsz, seq, C_WIDTH)
    return out_c, out_l


def _moe(h, router_w, router_b, w_gu, b_gu, w_dn, b_dn):
    n, d = h.shape
    logits = (h @ router_w + router_b).astype(jnp.float32)
    top_val, top_idx = lax.top_k(logits, TOP_K)
    gate = jax.nn.softmax(top_val, axis=-1)
    n_assign = n * TOP_K
    flat_e = top_idx.reshape(-1).astype(jnp.int32)
    flat_tok = jnp.arange(n_assign, dtype=jnp.int32) // TOP_K
    flat_gate = gate.reshape(-1)
    order = jnp.argsort(flat_e)
    e_sorted = flat_e[order]
    counts = jnp.bincount(flat_e, length=N_EXPERTS)
    padded = (counts + MOE_BLOCK - 1) // MOE_BLOCK * MOE_BLOCK
    pad_end = jnp.cumsum(padded)
    pad_start = pad_end - padded
    start = jnp.cumsum(counts) - counts
    dest = pad_start[e_sorted] + jnp.arange(n_assign, dtype=jnp.int32) - start[e_sorted]
    n_blocks = (n_assign + N_EXPERTS * (MOE_BLOCK - 1) + MOE_BLOCK - 1) // MOE_BLOCK
    cap = n_blocks * MOE_BLOCK
    slot_tok = jnp.full((cap,), n, dtype=jnp.int32).at[dest].set(flat_tok[order])
    slot_gate = jnp.zeros((cap,), jnp.float32).at[dest].set(flat_gate[order])
    block_e = jnp.minimum(jnp.searchsorted(pad_end, jnp.arange(n_blocks, dtype=jnp.int32) * MOE_BLOCK, side='right'),
                          N_EXPERTS - 1)
    h_pad = jnp.concatenate([h, jnp.zeros((1, d), h.dtype)], axis=0)
    xb = h_pad[slot_tok].reshape(n_blocks, MOE_BLOCK, d)

    def expert_block(args):
        xblk, e = args
        gu = xblk @ w_gu[e] + b_gu[e]
        g_, u_ = gu[:, :D_FF], gu[:, D_FF:]
        g_ = jnp.minimum(g_, SWIGLU_LIMIT)
        u_ = jnp.clip(u_, -SWIGLU_LIMIT, SWIGLU_LIMIT)
        act = (u_ + 1) * (g_ * jax.nn.sigmoid(SWIGLU_ALPHA * g_))
        return act @ w_dn[e] + b_dn[e]

    yb = lax.map(expert_block, (xb, block_e)).reshape(cap, d)
    out = jnp.zeros((n + 1, d), yb.dtype).at[slot_tok].add(yb * slot_gate[:, None].astype(yb.dtype))
    return out[:n]


def setup_inputs(seed: int = 0) -> dict:
    key = jax.random.key(seed)
    keys = iter(jax.random.split(key, 40))

    def nrm(shape, scale):
        return jax.random.normal(next(keys), shape, jnp.float32) * scale

    def unif(shape, lo, hi):
        return jax.random.uniform(next(keys), shape, jnp.float32, lo, hi)

    L = DEPTH
    a_init = unif((L, 2, A_WIDTH), 0.9, 0.999)
    return {
        'x': nrm((BATCH, SEQ, D_MODEL), 1.0),
        'c': nrm((BATCH, D_MODEL), 1.0),
        'ctx': nrm((BATCH, CTX_LEN, D_MODEL), 1.0),
        'c_ctx': nrm((D_MODEL,), 1.0),
        'ada_w': nrm((L, D_MODEL, 6 * D_MODEL), 0.5 * D_MODEL ** -0.5),
        'ada_b': nrm((L, 6 * D_MODEL), 0.02),
        'norm_mix_g': 1.0 + nrm((L, D_MODEL), 0.1),
        'norm_ffn_g': 1.0 + nrm((L, D_MODEL), 0.1),
        'w_in': nrm((L, D_MODEL, IN_COLS), D_MODEL ** -0.5),
        'w_out': nrm((L, D_MIX, D_MODEL), D_MIX ** -0.5),
        'lru_conv_w': nrm((L, 2, A_CONV, A_WIDTH), A_CONV ** -0.5),
        'lru_conv_b': nrm((L, 2, A_WIDTH), 0.02),
        'lru_wr': nrm((L, 2, A_HEADS, A_HD, A_HD), A_HD ** -0.5),
        'lru_br': nrm((L, 2, A_WIDTH), 0.1),
        'lru_wi': nrm((L, 2, A_HEADS, A_HD, A_HD), A_HD ** -0.5),
        'lru_bi': nrm((L, 2, A_WIDTH), 0.1),
        'lru_lambda': jnp.log(a_init) - jnp.log1p(-a_init),
        'rwkv_mu': unif((L, 2, B_COLS), 0.0, 0.5),
        'rwkv_w0': unif((L, 2, B_WIDTH), -6.0, -1.0),
        'rwkv_w2': nrm((L, 2, DECAY_RANK, B_WIDTH), 0.5 * DECAY_RANK ** -0.5),
        'rwkv_a0': nrm((L, 2, B_WIDTH), 0.3),
        'rwkv_a2': nrm((L, 2, ICLR_RANK, B_WIDTH), 0.5 * ICLR_RANK ** -0.5),
        'rwkv_g2': nrm((L, GATE_RANK, B_WIDTH), GATE_RANK ** -0.5),
        'rwkv_kk': 0.85 + nrm((L, B_WIDTH), 0.05),
        'rwkv_ka': 1.0 + nrm((L, B_WIDTH), 0.05),
        'rwkv_rk': nrm((L, B_HEADS, B_HD), 0.1),
        'rwkv_lnx_g': 1.0 + nrm((L, B_WIDTH), 0.1),
        'rwkv_lnx_b': nrm((L, B_WIDTH), 0.02),
        'na_rpb': nrm((L, C_HEADS, 2 * WIN_H - 1, 2 * WIN_W - 1), 0.2),
        'router_w': nrm((L, D_MODEL, N_EXPERTS), D_MODEL ** -0.5),
        'router_b': nrm((L, N_EXPERTS), 0.01),
        'moe_w_gu': nrm((L, N_EXPERTS, D_MODEL, 2 * D_FF), D_MODEL ** -0.5),
        'moe_b_gu': nrm((L, N_EXPERTS, 2 * D_FF), 0.02),
        'moe_w_dn': nrm((L, N_EXPERTS, D_FF, D_MODEL), D_FF ** -0.5),
        'moe_b_dn': nrm((L, N_EXPERTS, D_MODEL), 0.02),
        'final_g': 1.0 + nrm((D_MODEL,), 0.1),
    }


def reference(x, c, ctx, c_ctx, ada_w, ada_b, norm_mix_g, norm_ffn_g, w_in, w_out,
              lru_conv_w, lru_conv_b, lru_wr, lru_br, lru_wi, lru_bi, lru_lambda,
              rwkv_mu, rwkv_w0, rwkv_w2, rwkv_a0, rwkv_a2, rwkv_g2, rwkv_kk, rwkv_ka, rwkv_rk,
              rwkv_lnx_g, rwkv_lnx_b, na_rpb, router_w, router_b, moe_w_gu, moe_b_gu,
              moe_w_dn, moe_b_dn, final_g):
    bsz, seq, d = x.shape
    ctx_len = ctx.shape[1]
    cond_lat = jax.nn.silu(c)[:, None, :]
    cond_ctx = jax.nn.silu(c_ctx)[None, None, :]
    xl, xc = x, ctx
    for l in range(DEPTH):
        last = l == DEPTH - 1
        mod_l = jnp.split(cond_lat @ ada_w[l] + ada_b[l], 6, axis=-1)
        mod_c = jnp.split(cond_ctx @ ada_w[l] + ada_b[l], 6, axis=-1)
        hl = _modulate(_rmsnorm(xl, norm_mix_g[l]), mod_l[0], mod_l[1])
        hc = _modulate(_rmsnorm(xc, norm_mix_g[l]), mod_c[0], mod_c[1])
        pl = hl @ w_in[l]
        pc = hc @ w_in[l]
        pl_a, pl_b, pl_c = jnp.split(pl, [A_COLS, A_COLS + B_COLS], axis=-1)
        pc_a, pc_b, pc_c = jnp.split(pc, [A_COLS, A_COLS + B_COLS], axis=-1)
        need_ctx = not last
        ya_c, ya_l = _lru_mixer(pc_a, pl_a, lru_conv_w[l], lru_conv_b[l], lru_wr[l], lru_br[l],
                                lru_wi[l], lru_bi[l], lru_lambda[l], need_ctx)
        yb_c, yb_l = _rwkv_mixer(pc_b, pl_b, rwkv_mu[l], rwkv_w0[l], rwkv_w2[l], rwkv_a0[l], rwkv_a2[l],
                                 rwkv_g2[l], rwkv_kk[l], rwkv_ka[l], rwkv_rk[l], rwkv_lnx_g[l],
                                 rwkv_lnx_b[l], need_ctx)
        yc_c, yc_l = _na_mixer(pc_c, pl_c, na_rpb[l], need_ctx)
        xl = xl + mod_l[2] * (jnp.concatenate([ya_l, yb_l, yc_l], axis=-1) @ w_out[l])
        hl = _modulate(_rmsnorm(xl, norm_ffn_g[l]), mod_l[3], mod_l[4])
        if last:
            yl = _moe(hl.reshape(-1, d), router_w[l], router_b[l], moe_w_gu[l], moe_b_gu[l],
                      moe_w_dn[l], moe_b_dn[l])
        else:
            xc = xc + mod_c[2] * (jnp.concatenate([ya_c, yb_c, yc_c], axis=-1) @ w_out[l])
            hc = _modulate(_rmsnorm(xc, norm_ffn_g[l]), mod_c[3], mod_c[4])
            tok = jnp.concatenate([hc.reshape(-1, d), hl.reshape(-1, d)], axis=0)
            y = _moe(tok, router_w[l], router_b[l], moe_w_gu[l], moe_b_gu[l], moe_w_dn[l], moe_b_dn[l])
            xc = xc + mod_c[5] * y[:bsz * ctx_len].reshape(bsz, ctx_len, d)
            yl = y[bsz * ctx_len:]
        xl = xl + mod_l[5] * yl.reshape(bsz, seq, d)
    return _rmsnorm(xl, final_g)
```

```python
import contextlib

import numpy as np
import concourse.bass as bass
import concourse.mybir as mybir
from concourse.bass_utils import run_bass_kernel_spmd

F32 = mybir.dt.float32
BF16 = mybir.dt.bfloat16
AF = mybir.ActivationFunctionType
ALU = mybir.AluOpType
AX = mybir.AxisListType

D = 1024
NCH = 8
CTX = 256
OWN = 2048
HALO = 256
NTOK = CTX + OWN
NEXT = CTX + OWN + HALO
CTX0 = 1
LAT0 = 258
HAL0 = LAT0 + OWN
NTP = HAL0 + HALO
IN_COLS = 3008
XC0 = 3
XL0 = 262
NXA = 2320
SEM_ROT = 30000


class Sched:
    def __init__(self, nc, es, n_dma_sems=12):
        self.nc = nc
        self.es = es
        self.engs = {"pe": nc.tensor, "act": nc.scalar, "dve": nc.vector,
                     "pool": nc.gpsimd, "sp": nc.sync}
        self.sem_id = 0
        self.cur_sem = {}
        self.cnt = {}
        for e in self.engs:
            self._new_sem(e)
        self.seen = {e: {} for e in self.engs}
        self.last_w = {}
        self.readers = {}
        self.dma_sems = {}
        self.dma_idx = {}
        for q in ("sp", "pool", "act"):
            self.dma_sems[q] = [self._alloc_sem(f"d{q}{i}") for i in range(n_dma_sems)]
            self.dma_idx[q] = 0
        self.dma_val = {}
        self.n_wait = 0
        self.n_ins = 0
        self.pending = {e: False for e in self.engs}

    def _alloc_sem(self, name):
        self.sem_id += 1
        return self.es.enter_context(self.nc.semaphore(f"{name}_{self.sem_id}"))

    def _new_sem(self, e):
        self.cur_sem[e] = self._alloc_sem("s" + e)
        self.cnt[e] = 0

    def _wait(self, e, ev):
        sem, val = ev
        k = id(sem)
        if self.seen[e].get(k, 0) >= val:
            return
        self.engs[e].wait_ge(sem, val)
        self.n_wait += 1
        self.seen[e][k] = val

    def _deps(self, reads, writes):
        evs = []
        for r in reads:
            w = self.last_w.get(r)
            if w is not None:
                evs.append(w)
        for w_ in writes:
            w = self.last_w.get(w_)
            if w is not None:
                evs.append(w)
            evs.extend(self.readers.get(w_, ()))
        return evs

    def _record(self, ev, reads, writes):
        for r in reads:
            self.readers.setdefault(r, []).append(ev)
        for w_ in writes:
            self.last_w[w_] = ev
            self.readers[w_] = []

    def op(self, e, fn, reads=(), writes=(), inc=True):
        own = self.cur_sem[e]
        for ev in self._deps(reads, writes):
            if ev[0] is own and e == "pe":
                continue
            if ev[0] is own and ev[1] > self.cnt[e]:
                continue
            self._wait(e, ev)
        ins = fn()
        self.n_ins += 1
        ev = (own, self.cnt[e] + 1)
        if inc:
            ins.then_inc(own, 1)
            self.cnt[e] += 1
            self.pending[e] = False
            if self.cnt[e] >= SEM_ROT:
                self._new_sem(e)
        else:
            self.pending[e] = True
        self._record(ev, reads, writes)
        return ins

    def dma(self, q, out, in_, reads=(), writes=(), **kw):
        for ev in self._deps(reads, writes):
            self._wait(q, ev)
        i = self.dma_idx[q]
        self.dma_idx[q] = (i + 1) % len(self.dma_sems[q])
        sem = self.dma_sems[q][i]
        prev = self.dma_val.get(id(sem), 0)
        if prev:
            self._wait(q, (sem, prev))
        ins = self.engs[q].dma_start(out=out, in_=in_, **kw)
        ins.then_inc(sem, 16)
        self.n_ins += 1
        val = prev + 16
        self.dma_val[id(sem)] = val
        ev = (sem, val)
        self._record(ev, reads, writes)
        return ev

    def flush(self, e):
        if self.pending[e]:
            own = self.cur_sem[e]
            self.engs[e].nop().then_inc(own, 1)
            self.cnt[e] += 1
            self.pending[e] = False

    def barrier(self):
        for e in self.engs:
            self.flush(e)
        evs = []
        for e in self.engs:
            if self.cnt[e] > 0:
                evs.append((self.cur_sem[e], self.cnt[e]))
        for q in self.dma_sems:
            for sem in self.dma_sems[q]:
                v = self.dma_val.get(id(sem), 0)
                if v:
                    evs.append((sem, v))
        for e in self.engs:
            for ev in evs:
                if ev[0] is self.cur_sem[e] and e == "pe":
                    continue
                self._wait(e, ev)
        self.last_w = {}
        self.readers = {}


INPUT_SHAPES = {
    "xT": [D, NEXT],
    "cvec": [128, 8, 2],
    "ada_w": [D, 6 * D],
    "ada_b": [128, 48],
    "g_mix": [128, 8],
    "g_ffn": [128, 8],
    "w_in": [D, IN_COLS],
    "w_out": [D, D],
    "lru_cw": [128, 2, 2, 4],
    "lru_cb": [128, 2, 2],
    "lru_br": [128, 2, 2],
    "lru_bi": [128, 2, 2],
    "lru_lam": [128, 2, 2],
    "lru_wr": [2, 4, 64, 64],
    "lru_wi": [2, 4, 64, 64],
    "rwkv_mu": [2, 960],
    "rwkv_w0": [128, 2, 2],
    "rwkv_a0": [128, 2, 2],
    "rwkv_w2": [2, 64, 256],
    "rwkv_a2": [2, 64, 256],
    "rwkv_g2": [64, 256],
    "rwkv_kk": [128, 2],
    "rwkv_ka": [128, 2],
    "rwkv_rk": [128, 2],
    "rwkv_lng": [128, 2],
    "rwkv_lnb": [128, 2],
    "na_tab": [3, 8, 640, 128],
    "router_w": [D, 32],
    "router_b": [1, 32],
    "moe_w_gu": [32, D, 2 * D],
    "moe_b_gu": [128, 32, 16],
    "moe_w_dn": [32, D, D],
    "moe_b_dn": [32, D],
    "final_g": [128, 8],
    "c_ident": [128, 128],
    "c_sel": [32, 32 * 128],
    "c_masks": [64, 3, 64],
    "oh_self": [128, 2],
    "oh_part": [128, 2],
    "lru_h_in": [128, 2],
    "rwkv_H_in": [64, 4, 64],
}


GLOBAL_INPUTS = ("xT", "cvec", "final_g", "c_ident", "c_sel", "c_masks", "lru_h_in", "rwkv_H_in", "oh_self", "oh_part")


class _LazyIns(dict):
    def __init__(self, prog):
        super().__init__()
        self.prog = prog

    def __getitem__(self, name):
        if self.prog.fused and name not in GLOBAL_INPUTS:
            real = f"{name}_L{self.prog.layer}"
        else:
            real = name
        if real not in self:
            ap = self.prog.nc.dram_tensor(real, list(INPUT_SHAPES[name]), F32, kind="ExternalInput").ap()
            dict.__setitem__(self, real, ap)
        return dict.__getitem__(self, real)


class Prog:
    def __init__(self, dbg=()):
        self.nc = bass.Bass("TRN2", target_bir_lowering=False)
        self.dbg = set(dbg)
        self.ins = _LazyIns(self)
        self.outs = {}
        self.fused = False
        self.layer = 0
        self.x_src = None
        self.skip_ctx_out = False
        self.tok_tiles = [(0, 256, 1)] + [(256 + i * 512, 512, 0) for i in range(4)]

    def din(self, name, shape, dt=F32):
        ap = self.nc.dram_tensor(name, list(shape), dt, kind="ExternalInput").ap()
        self.ins[name] = ap
        return ap

    def dout(self, name, shape, dt=F32):
        ap = self.nc.dram_tensor(name, list(shape), dt, kind="ExternalOutput").ap()
        self.outs[name] = ap
        return ap

    def sb(self, st, name, shape, dt=F32):
        return st.enter_context(self.nc.sbuf_tensor(f"{name}_l{self.layer}", list(shape), dt)).ap()

    def xchg(self, tag, src, dst, P_, F_, src_key, dst_key):
        nc, S = self.nc, self.S
        with contextlib.ExitStack() as st:
            ctr = self.sb(st, f"xc_{tag}_c", [P_, 2, F_]); g = self.sb(st, f"xc_{tag}_g", [P_, 2, F_])
            din_ = nc.dram_tensor(f"cc_in_{tag}", [P_, 2 * F_], F32)
            dou = nc.dram_tensor(f"cc_out_{tag}", [P_, 2 * F_], F32)
            ck, gk = ("xc_c", tag), ("xc_g", tag)
            for k in range(2):
                self.TS("dve", ctr[:, k, :], src, self.ohs[0:P_, k:k + 1], None, ALU.mult, None, [src_key, "ohs"], [ck])
            S.dma("pool", din_.ap(), ctr.rearrange("p a b -> p (a b)"), reads=[ck], writes=[("ccin", tag)])
            S.op("pool", lambda: nc.gpsimd.collective_compute("AllReduce", ALU.add, replica_groups=[[0, 1], [2, 3], [4, 5], [6, 7]],
                                                             ins=[din_.ap().opt()], outs=[dou.ap().opt()]),
                 [("ccin", tag)], [("ccout", tag)])
            S.dma("pool", g.rearrange("p a b -> p (a b)"), dou.ap(), reads=[("ccout", tag)], writes=[gk])
            self.TS("dve", dst, g[:, 0, :], self.ohp[0:P_, 0:1], None, ALU.mult, None, [gk, "ohp"], [dst_key])
            self.STT(dst, g[:, 1, :], self.ohp[0:P_, 1:2], dst, ALU.mult, ALU.add, [gk, "ohp", dst_key], [dst_key])
            S.barrier()

    def declare_inputs(self):
        pass

    def A(self, e, out, in_, func, reads, writes, **kw):
        nc = self.nc
        return self.S.op(e, lambda: nc.scalar.activation(out=out, in_=in_, func=func, **kw), reads, writes)

    def eng(self, e):
        return {"dve": self.nc.vector, "pool": self.nc.gpsimd}[e]

    def TT(self, e, out, in0, in1, op, reads, writes):
        en = self.eng(e)
        return self.S.op(e, lambda: en.tensor_tensor(out=out, in0=in0, in1=in1, op=op), reads, writes)

    def TS(self, e, out, in0, s1, s2, op0, op1, reads, writes):
        en = self.eng(e)
        if op1 is None:
            return self.S.op(e, lambda: en.tensor_scalar(out=out, in0=in0, scalar1=s1, scalar2=None, op0=op0), reads, writes)
        return self.S.op(e, lambda: en.tensor_scalar(out=out, in0=in0, scalar1=s1, scalar2=s2, op0=op0, op1=op1), reads, writes)

    def STT(self, out, in0, scalar, in1, op0, op1, reads, writes):
        nc = self.nc
        return self.S.op("dve", lambda: nc.vector.scalar_tensor_tensor(out=out, in0=in0, scalar=scalar, in1=in1, op0=op0, op1=op1), reads, writes)

    def MM(self, out, lhsT, rhs, start, stop, reads, writes, inc=None):
        nc = self.nc
        if inc is None:
            inc = stop
        return self.S.op("pe", lambda: nc.tensor.matmul(out, lhsT, rhs, start=start, stop=stop), reads, writes, inc=inc)

    def TR(self, out, in_, ident, reads, writes, inc=True):
        nc = self.nc
        return self.S.op("pe", lambda: nc.tensor.transpose(out, in_, ident), reads, writes, inc=inc)

    def CP(self, e, out, in_, reads, writes):
        nc = self.nc
        if e == "act":
            return self.S.op("act", lambda: nc.scalar.activation(out=out, in_=in_, func=AF.Copy), reads, writes)
        en = self.eng(e)
        return self.S.op(e, lambda: en.tensor_copy(out=out, in_=in_), reads, writes)

    def MS(self, e, ap, val, writes):
        en = self.eng(e)
        return self.S.op(e, lambda: en.memset(ap, val), (), writes)

    def dump(self, name, ap_sbuf, shape, reads):
        if name not in self.dbg:
            return
        o = self.dout("dbg_" + name, shape)
        self.S.dma("sp", o, ap_sbuf, reads=reads)

    def setup(self, es):
        nc = self.nc
        self.S = Sched(nc, es)
        S = self.S
        I = self.ins
        self.ps = [nc.alloc_psum_tensor(f"ps{i}", [128, 1024], F32).ap() for i in range(4)]
        sb = lambda n, sh, dt=F32: self.sb(es, n, sh, dt)
        self.ident = sb("ident", [128, 128])
        S.dma("sp", self.ident, I["c_ident"], writes=["ident"])
        self.onesd = sb("onesd", [128, 128])
        self.MS("pool", self.onesd, 1.0 / D, ["onesd"])
        self.bd1 = sb("bd1", [128, 128])
        self.MS("pool", self.bd1, 0.0, ["bd1"])
        self.MS("pool", self.bd1[0:64, 0:64], 1.0, ["bd1"])
        self.MS("pool", self.bd1[64:128, 64:128], 1.0, ["bd1"])
        self.eps6 = sb("eps6", [128, 1])
        self.MS("pool", self.eps6, 1e-6, ["eps6"])
        self.mod = sb("mod", [128, 48, 2])
        self.gs1 = sb("gs1", [128, 8, 2]); self.gs2 = sb("gs2", [128, 8, 2])
        self.gmix = sb("gmix", [128, 8]); self.gffn = sb("gffn", [128, 8]); self.gfin = sb("gfin", [128, 8])
        S.dma("sp", self.gfin, I["final_g"], writes=["gfin"])
        if self.fused:
            self.ohs = sb("ohs", [128, 2]); self.ohp = sb("ohp", [128, 2])
            S.dma("sp", self.ohs, I["oh_self"], writes=["ohs"])
            S.dma("sp", self.ohp, I["oh_part"], writes=["ohp"])

    def adaln(self):
        nc, S, I = self.nc, self.S, self.ins
        with contextlib.ExitStack() as st:
            cond = self.sb(st, "cond", [128, 8, 2])
            adab = self.sb(st, "adab", [128, 48])
            wbuf = [self.sb(st, f"adaw{i}", [128, 8, D]) for i in range(2)]
            S.dma("sp", self.gmix, I["g_mix"], writes=["gmix"])
            S.dma("sp", self.gffn, I["g_ffn"], writes=["gffn"])
            S.dma("sp", cond, I["cvec"], writes=["cond"])
            S.dma("sp", adab, I["ada_b"], writes=["adab"])
            self.A("act", cond, cond, AF.Silu, ["cond"], ["cond"])
            pst = self.ps[0][:, 0:96]
            for k in range(6):
                wb = wbuf[k % 2]
                wk = ("adaw", k % 2)
                S.dma("sp", wb, I["ada_w"][:, k * D:(k + 1) * D].rearrange("(c p) n -> p c n", p=128), writes=[wk])
                for fc in range(8):
                    col = (k * 8 + fc) * 2
                    for dc in range(8):
                        self.MM(pst[:, col:col + 2], wb[:, dc, fc * 128:(fc + 1) * 128], cond[:, dc, :],
                                dc == 0, dc == 7, [wk, "cond"], [("ps", 0, 0)])
            mod2 = self.mod.rearrange("p k j -> p (k j)")
            b3 = bass.AP(adab.tensor, adab.offset, [list(adab.ap[0]), [1, 48], [0, 2]])
            self.TT("dve", self.mod, pst.rearrange("p (k j) -> p k j", j=2), b3, ALU.add, [("ps", 0, 0), "adab"], ["mod"])
            for (gs, g, k, nm) in ((self.gs1, self.gmix, 1, "gs1"), (self.gs2, self.gffn, 4, "gs2")):
                g3 = bass.AP(g.tensor, g.offset, [list(g.ap[0]), [1, 8], [0, 2]])
                self.TS("dve", gs, self.mod[:, k * 8:(k + 1) * 8, :], 1.0, None, ALU.add, None, ["mod"], [nm])
                self.TT("dve", gs, gs, g3, ALU.mult, [nm, "gmix", "gffn"], [nm])
            self.dump("mod", self.mod, [128, 48, 2], ["mod"])
            S.barrier()

    def norm_tile(self, xt, xkey, T, gs, shift_k, j, out_fn, tag):
        nc, S = self.nc, self.S
        ps = self.ps[1][:, 0:T]
        pk = ("ps", 1, 0)
        for c in range(8):
            sq = self.nsq[c % 2][:, 0:T]
            self.A("act", sq, xt[:, c, :], AF.Square, [xkey], [("nsq", c % 2)])
            self.MM(ps, self.onesd, sq, c == 0, c == 7, ["onesd", ("nsq", c % 2)], [pk], inc=True)
        rstd = self.nrstd[:, 0:T]
        self.A("act", rstd, ps, AF.Sqrt, [pk, "eps6"], ["nrstd"], bias=self.eps6[:, 0:1], scale=1.0)
        S.op("dve", lambda: nc.vector.reciprocal(out=rstd, in_=rstd), ["nrstd"], ["nrstd"])
        for c in range(8):
            tmp = self.ntmp[c % 2][:, 0:T]
            self.TT("pool" if c % 2 else "dve", tmp, xt[:, c, :], rstd, ALU.mult, [xkey, "nrstd"], [("ntmp", c % 2)])
            out_fn(c, tmp, ("ntmp", c % 2))

    def norm1(self, hT):
        nc, S, I = self.nc, self.S, self.ins
        with contextlib.ExitStack() as st:
            xb = [self.sb(st, f"n1x{i}", [128, 8, 512]) for i in range(2)]
            self.nsq = [self.sb(st, f"nsq{i}", [128, 512]) for i in range(2)]
            self.ntmp = [self.sb(st, f"ntmp{i}", [128, 512]) for i in range(2)]
            self.nrstd = self.sb(st, "nrstd", [128, 512])
            for c in range(8):
                self.MS("pool", hT[:, c, 0:1], 0.0, ["hT"])
                self.MS("pool", hT[:, c, 257:258], 0.0, ["hT"])
            tiles = [(0, 256, CTX0, 1)] + [(CTX + i * 512, 512, LAT0 + i * 512, 0) for i in range(4)] + [(CTX + OWN, 256, HAL0, 0)]
            xv = self.x_src.rearrange("(c p) n -> p c n", p=128)
            for ti, (x0, T, h0, j) in enumerate(tiles):
                xt = xb[ti % 2]
                xk = ("n1x", ti % 2)
                S.dma("sp", xt[:, :, 0:T], xv[:, :, x0:x0 + T], writes=[xk])

                def out_fn(c, tmp, tk, h0=h0, T=T, j=j):
                    self.A("act", hT[:, c, h0:h0 + T], tmp, AF.Identity, [tk, "gs1", "mod"], ["hT"],
                           scale=self.gs1[:, c, j:j + 1], bias=self.mod[:, 0 * 8 + c, j:j + 1])
                self.norm_tile(xt[:, :, 0:T], xk, T, self.gs1, 0, j, out_fn, "n1")
            S.barrier()

    def load_w_bf16(self, dst, col0, ncols, key):
        src = self.ins["w_in"][:, col0:col0 + ncols].rearrange("(c p) n -> p c n", p=128)
        self.S.dma("pool", dst, src, writes=[key])

    def proj(self, ps, pkey, w, wkey, wcol0, M, hT, hcol0, T):
        for kc in range(8):
            self.MM(ps[0:M, 0:T], w[:, kc, wcol0:wcol0 + M], hT[:, kc, hcol0:hcol0 + T], kc == 0, kc == 7,
                    [wkey, "hT"], [pkey])

    def lru_setup(self, st):
        S, I = self.S, self.ins
        sb = lambda n, sh, dt=F32: self.sb(st, n, sh, dt)
        L = {}
        L["cw"] = sb("l_cw", [128, 2, 2, 4])
        S.dma("sp", L["cw"], I["lru_cw"], writes=["l_par"])
        for nm in ("cb", "br", "bi", "lam"):
            L[nm] = sb("l_" + nm, [128, 2, 2])
            S.dma("sp", L[nm], I["lru_" + nm], writes=["l_par"])
        L["wr"] = sb("l_wr", [128, 2, 2, 128]); L["wi"] = sb("l_wi", [128, 2, 2, 128])
        for nm in ("wr", "wi"):
            self.MS("pool", L[nm], 0.0, ["l_" + nm])
            for di in range(2):
                for blk in range(4):
                    cc, hh = blk // 2, blk % 2
                    S.dma("sp", L[nm][64 * hh:64 * hh + 64, di, cc, 64 * hh:64 * hh + 64], I["lru_" + nm][di, blk],
                          writes=["l_" + nm])
        L["c8"] = sb("l_c8", [128, 2, 2]); L["c16"] = sb("l_c16", [128, 2, 2])
        self.A("act", L["c8"], L["lam"], AF.Exp, ["l_par"], ["l_c8"], scale=-1.0)
        self.A("act", L["c8"], L["c8"], AF.Ln, ["l_c8"], ["l_c8"], bias=1.0, scale=1.0)
        self.TS("dve", L["c16"], L["c8"], -16.0, None, ALU.mult, None, ["l_c8"], ["l_c16"])
        self.TS("dve", L["c8"], L["c8"], -8.0, None, ALU.mult, None, ["l_c8"], ["l_c8"])
        L["hin"] = sb("l_hin", [128, 2])
        if not self.fused:
            S.dma("sp", L["hin"], I["lru_h_in"], writes=["l_hin"])
        L["w"] = sb("l_w", [128, 8, 512], BF16)
        self.load_w_bf16(L["w"], 0, 512, "l_w")
        self.L = L

    def lru_xa(self, st, hT):
        xa = self.sb(st, "l_xa", [128, 2, NXA])
        for cc in range(2):
            for (c0, n) in ((0, 3), (XC0 + 256, 3), (XL0 + 2048 + 3, NXA - (XL0 + 2048 + 3))):
                self.MS("pool", xa[:, cc, c0:c0 + n], 0.0, ["l_xa"])
        tiles = [(CTX0, 256, XC0)] + [(LAT0 + i * 512, 512, XL0 + i * 512) for i in range(4)] + [(HAL0, 3, XL0 + 2048)]
        i = 0
        for (h0, T, x0) in tiles:
            for cc in range(2):
                ps = self.ps[2 + (i % 2)]
                pk = ("ps", 2 + (i % 2), 0)
                self.proj(ps, pk, self.L["w"], "l_w", cc * 128, 128, hT, h0, T)
                self.CP("act" if i % 2 else "dve", xa[:, cc, x0:x0 + T], ps[:, 0:T], [pk], ["l_xa"])
                i += 1
        return xa

    def lru_pass(self, st, di, xa, hT, h1, mixT):
        nc, S, L = self.nc, self.S, self.L
        sb = lambda n, sh, dt=F32: self.sb(st, n, sh, dt)
        tg = f"lp{di}"
        nb = 2
        bufs = {nm: [sb(f"{tg}{nm}{i}", [128, 512]) for i in range(nb)] for nm in ("u", "r", "i", "a", "b", "h", "g")}
        sgn = 1 if di == 1 else -1
        seqs = [(XC0, 256, 0, CTX0), (XL0, 2048, 256, LAT0)]
        if di == 1:
            seqs = seqs[::-1]
            if self.skip_ctx_out:
                seqs = seqs[:1]
        it = 0
        for cc in range(2):
            carry = None
            for (x0, n, m0, h0) in seqs:
                T = min(512, n)
                tl = list(range(n // T))
                if di == 1:
                    tl = tl[::-1]
                    carry = L["hin"][:, cc:cc + 1] if x0 == XL0 else None
                elif x0 == XC0:
                    carry = None
                for ti in tl:
                    k = it % nb
                    kp = (it - 1) % nb
                    it += 1
                    c0 = x0 + ti * T
                    mc = m0 + ti * T
                    K_ = {nm: ("l" + nm, di, k) for nm in bufs}
                    ut, rt, itl, at, bt, ht, gt = (bufs[nm][k][:, 0:T] for nm in ("u", "r", "i", "a", "b", "h", "g"))
                    self.A("act", ut, xa[:, cc, c0:c0 + T], AF.Identity, ["l_xa", "l_par"], [K_["u"]],
                           scale=L["cw"][:, di, cc, 0:1], bias=L["cb"][:, di, cc:cc + 1])
                    for j in range(1, 4):
                        self.STT(ut, xa[:, cc, c0 + sgn * j:c0 + sgn * j + T], L["cw"][:, di, cc, j:j + 1], ut,
                                 ALU.mult, ALU.add, ["l_xa", "l_par", K_["u"]], [K_["u"]])
                    psr = self.ps[0]; psi = self.ps[1]
                    self.MM(psr[:, 0:T], L["wr"][:, di, cc, :], ut, True, True, ["l_wr", K_["u"]], [("ps", 0, 0)])
                    self.MM(psi[:, 0:T], L["wi"][:, di, cc, :], ut, True, True, ["l_wi", K_["u"]], [("ps", 1, 0)])
                    self.A("act", rt, psr[:, 0:T], AF.Sigmoid, [("ps", 0, 0), "l_par"], [K_["r"]], bias=L["br"][:, di, cc:cc + 1], scale=1.0)
                    self.A("act", itl, psi[:, 0:T], AF.Sigmoid, [("ps", 1, 0), "l_par"], [K_["i"]], bias=L["bi"][:, di, cc:cc + 1], scale=1.0)
                    self.A("act", at, rt, AF.Exp, [K_["r"], "l_c8"], [K_["a"]], scale=L["c8"][:, di, cc:cc + 1])
                    self.A("act", bt, rt, AF.Exp, [K_["r"], "l_c16"], [K_["b"]], scale=L["c16"][:, di, cc:cc + 1])
                    self.A("act", bt, bt, AF.Sqrt, [K_["b"]], [K_["b"]], scale=-1.0, bias=1.0)
                    self.TT("pool", itl, itl, ut, ALU.mult, [K_["i"], K_["u"]], [K_["i"]])
                    self.TT("pool", bt, bt, itl, ALU.mult, [K_["b"], K_["i"]], [K_["b"]])
                    init = 0.0 if carry is None else carry
                    if di == 0:
                        hdst = h1[:, cc, mc:mc + T]
                        S.op("dve", lambda hdst=hdst, at=at, bt=bt, init=init: nc.vector.tensor_tensor_scan(
                            out=hdst, data0=at, data1=bt, initial=init, op0=ALU.mult, op1=ALU.add),
                            [K_["a"], K_["b"], "l_h1"], ["l_h1"])
                        carry = h1[:, cc, mc + T - 1:mc + T]
                    else:
                        S.op("dve", lambda ht=ht, at=at, bt=bt, init=init: nc.vector.tensor_tensor_scan(
                            out=ht[:, ::-1], data0=at[:, ::-1], data1=bt[:, ::-1], initial=init, op0=ALU.mult, op1=ALU.add),
                            [K_["a"], K_["b"], "l_hin", ("lh", di, kp)], [K_["h"]])
                        carry = ht[:, 0:1]
                        psg = self.ps[2]
                        self.proj(psg, ("ps", 2, 0), L["w"], "l_w", 256 + cc * 128, 128, hT, h0 + ti * T, T)
                        self.A("act", gt, psg[:, 0:T], AF.Gelu, [("ps", 2, 0)], [K_["g"]])
                        self.TT("dve", bt, ht, h1[:, cc, mc:mc + T], ALU.add, [K_["h"], "l_h1", K_["b"]], [K_["b"]])
                        self.TT("pool", mixT[:, cc, mc:mc + T], gt, bt, ALU.mult, [K_["g"], K_["b"]], ["mixT"])

    def lru_stage(self, hT, mixT, phase):
        S = self.S
        with contextlib.ExitStack() as st:
            self.lru_setup(st)
            xa = self.lru_xa(st, hT)
            h1 = self.sb(st, "l_h1", [128, 2, NTOK])
            with contextlib.ExitStack() as st2:
                self.lru_pass(st2, 0, xa, hT, h1, mixT)
                S.barrier()
            if self.fused:
                hst = self.sb(st, "l_hst", [128, 2])
                for cc in range(2):
                    self.CP("dve", hst[:, cc:cc + 1], h1[:, cc, NTOK - 1:NTOK], ["l_h1"], ["l_hst"])
                self.xchg(f"lru{self.layer}", hst, self.L["hin"], 128, 2, "l_hst", "l_hin")
            if phase == "A":
                o = self.dout("lru_h_out", [128, 2])
                for cc in range(2):
                    S.dma("sp", o[:, cc:cc + 1], h1[:, cc, NTOK - 1:NTOK], reads=["l_h1"], allow_slow_non_contiguous=True)
            else:
                with contextlib.ExitStack() as st2:
                    self.lru_pass(st2, 1, xa, hT, h1, mixT)
                    S.barrier()
            S.barrier()

    def rwkv_setup(self, st):
        S, I, nc = self.S, self.ins, self.nc
        sb = lambda n, sh, dt=F32: self.sb(st, n, sh, dt)
        R = {}
        for nm in ("w0", "a0"):
            R[nm] = sb("r_" + nm, [128, 2, 2])
            S.dma("sp", R[nm], I["rwkv_" + nm], writes=["r_par"])
        for nm in ("kk", "ka", "rk", "lng", "lnb"):
            R[nm] = sb("r_" + nm, [128, 2])
            S.dma("sp", R[nm], I["rwkv_" + nm], writes=["r_par"])
        R["omka"] = sb("r_omka", [128, 2]); R["omka2"] = sb("r_omka2", [128, 2])
        self.TS("dve", R["omka"], R["ka"], -1.0, 1.0, ALU.mult, ALU.add, ["r_par"], ["r_omka"])
        self.TS("dve", R["omka2"], R["omka"], 2.0, None, ALU.mult, None, ["r_omka"], ["r_omka2"])
        R["lw"] = sb("r_lw", [128, 2, 256], BF16)
        for di in range(2):
            S.dma("pool", R["lw"][0:64, di, :], I["rwkv_w2"][di], writes=["r_lw"])
            S.dma("pool", R["lw"][64:128, di, :], I["rwkv_a2"][di], writes=["r_lw"])
        R["g2"] = sb("r_g2", [64, 256], BF16)
        S.dma("pool", R["g2"], I["rwkv_g2"], writes=["r_g2"])
        R["msk"] = sb("r_msk", [64, 3, 64])
        S.dma("sp", R["msk"], I["c_masks"], writes=["r_msk"])
        R["J"] = sb("r_J", [64, 64])
        self.CP("dve", R["J"], self.ident[0:64, 0:64][:, ::-1], ["ident"], ["r_J"])
        R["ones"] = sb("r_ones", [128, 64])
        self.MS("pool", R["ones"], 1.0, ["r_ones"])
        R["eps"] = sb("r_eps", [128, 1])
        self.MS("pool", R["eps"], 64e-5, ["r_eps"])
        R["Hin"] = sb("r_Hin", [64, 4, 64])
        if not self.fused:
            S.dma("sp", R["Hin"], I["rwkv_H_in"], writes=["r_Hin"])
        self.R = R

    def rwkv_stage0(self, st, hT, pb):
        S, I = self.S, self.ins
        sb = lambda n, sh, dt=F32: self.sb(st, n, sh, dt)
        wf = [sb(f"r0_wf{i}", [128, 8, 128]) for i in range(2)]
        mup = [sb(f"r0_mup{i}", [128, 128]) for i in range(2)]
        mun = [sb(f"r0_mun{i}", [128, 128]) for i in range(2)]
        c0 = [sb(f"r0_c0{i}", [128, 128]) for i in range(2)]
        Ws = [[sb(f"r0_W{i}{s}", [128, 8, 128], BF16) for s in range(3)] for i in range(2)]
        tiles = [(CTX0, 256, 0)] + [(LAT0 + i * 512, 512, 256 + i * 512) for i in range(4)]
        ip = 0
        for cc in range(8):
            k = cc % 2
            M = 128 if cc < 7 else 64
            col0 = 512 + cc * 128
            bc0 = cc * 128
            S.dma("sp", wf[k][:, :, 0:M], I["w_in"][:, col0:col0 + M].rearrange("(c p) n -> p c n", p=128), writes=[("r0wf", k)])
            for (t_, row, nm) in ((mup[k], 0, "r0mup"), (mun[k], 1, "r0mun")):
                src = I["rwkv_mu"][row:row + 1, bc0:bc0 + M]
                S.dma("sp", t_[:, 0:M], bass.AP(src.tensor, src.offset, [[0, 128], [1, M]]), writes=[(nm, k)])
            self.TT("dve", c0[k][:, 0:M], mup[k][:, 0:M], mun[k][:, 0:M], ALU.add, [("r0mup", k), ("r0mun", k)], [("r0c0", k)])
            self.TS("dve", c0[k][:, 0:M], c0[k][:, 0:M], -1.0, 1.0, ALU.mult, ALU.add, [("r0c0", k)], [("r0c0", k)])
            for s_, (sc, scn) in enumerate(((c0[k], "r0c0"), (mup[k], "r0mup"), (mun[k], "r0mun"))):
                sc3 = bass.AP(sc.tensor, sc.offset, [list(sc.ap[0]), [0, 8], [1, M]])
                self.TT("pool" if s_ == 1 else "dve", Ws[k][s_][:, :, 0:M], wf[k][:, :, 0:M], sc3, ALU.mult,
                        [("r0wf", k), (scn, k)], [("r0W", k, s_)])
            for (h0, T, p0) in tiles:
                ps = self.ps[2 + ip % 2]
                pk = ("ps", 2 + ip % 2, 0)
                ip += 1
                n = 0
                for s_, sh in enumerate((0, -1, 1)):
                    for kc in range(8):
                        self.MM(ps[0:M, 0:T], Ws[k][s_][:, kc, 0:M], hT[:, kc, h0 + sh:h0 + sh + T], n == 0, n == 23,
                                [("r0W", k, s_), "hT"], [pk])
                        n += 1
                dst = pb[:, cc, p0:p0 + T]
                if cc < 6:
                    self.CP("act" if ip % 2 else "dve", dst, ps[:, 0:T], [pk], ["r_pb"])
                elif cc == 6:
                    self.A("act", pb[0:64, cc, p0:p0 + T], ps[0:64, 0:T], AF.Tanh, [pk], ["r_pb"])
                    self.CP("dve", pb[64:128, cc, p0:p0 + T], ps[64:128, 0:T], [pk], ["r_pb"])
                else:
                    self.A("act", pb[0:64, cc, p0:p0 + T], ps[0:64, 0:T], AF.Sigmoid, [pk], ["r_pb"])

    def rwkv_pass(self, st, di, pb, y1, mixT, TB=128):
        nc, S, R = self.nc, self.S, self.R
        sb = lambda n, sh, dt=F32: self.sb(st, n, sh, dt)
        nC = TB // 64
        NB = nC * 4
        CL = 0.6065306597126334
        rev = di == 1
        tg = f"rp{di}_"
        names = ["sz", "a", "kk", "keff", "beta", "cs", "E1", "Em", "Ep", "Eh", "AT", "BT", "KT", "RT", "BhT", "KhT", "VT", "tmp", "tmp2"]
        if rev:
            names += ["a1", "ysum", "yc"]
        fm = {nm: [sb(tg + nm + str(hp), [128, TB]) for hp in range(2)] for nm in names}
        FK = lambda nm, hp: (tg + nm, hp)
        pc4 = sb(tg + "pc4", [64, 4, nC])
        opsT = {nm: [sb(tg + "o" + nm + str(h), [64, TB]) for h in range(4)] for nm in ("AT", "BT", "KT", "RT")}
        OK_ = lambda nm, h: (tg + "o" + nm, h)
        tok = sb(tg + "tok", [64, nC, 3, 2, 128])
        mats = {nm: sb(tg + nm, [64, NB, 64]) for nm in ("NT", "N", "Aak", "Gb", "Gk", "NT2", "N2", "Q", "Q2")}
        Xs = sb(tg + "Xs", [64, 256]); Us = sb(tg + "Us", [64, 256]); Ys = sb(tg + "Ys", [64, 256])
        H = sb(tg + "H", [64, 4, 64])
        y2 = sb(tg + "y2", [128, 2, TB])
        msk = R["msk"]
        ident64 = self.ident[0:64, 0:64]

        def v3(ap):
            a3 = ap.rearrange("p (c t) -> p c t", t=64)
            return a3[:, :, ::-1] if rev else a3

        if not rev:
            self.MS("pool", H, 0.0, [tg + "H"])
        else:
            self.CP("dve", H, R["Hin"], ["r_Hin"], [tg + "H"])
        tiles = [i * TB for i in range(NTOK // TB)]
        n_ctx_t = CTX // TB
        if rev:
            tiles = tiles[n_ctx_t:][::-1] + ([] if self.skip_ctx_out else tiles[:n_ctx_t][::-1])
        for tix, n0 in enumerate(tiles):
            if rev and tix == (NTOK - CTX) // TB:
                self.MS("pool", H, 0.0, [tg + "H"])
            th = pb[0:64, 6, n0:n0 + TB]; al = pb[64:128, 6, n0:n0 + TB]; sg = pb[0:64, 7, n0:n0 + TB]
            for hp in range(2):
                r_ = pb[:, 0 + hp, n0:n0 + TB]; k_ = pb[:, 2 + hp, n0:n0 + TB]; v_ = pb[:, 4 + hp, n0:n0 + TB]
                T_ = {nm: fm[nm][hp] for nm in names}
                pz = self.ps[0][:, 0:TB]; pa = self.ps[0][:, 512:512 + TB]; pss = self.ps[1][:, 0:TB]
                self.MM(pz, R["lw"][0:64, di, hp * 128:(hp + 1) * 128], th, True, True, ["r_lw", "r_pb"], [("ps", 0, 0)])
                self.A("act", T_["sz"], pz, AF.Sigmoid, [("ps", 0, 0), "r_par"], [FK("sz", hp)], bias=R["w0"][:, di, hp:hp + 1], scale=1.0)
                self.MM(pa, R["lw"][64:128, di, hp * 128:(hp + 1) * 128], al, True, True, ["r_lw", "r_pb"], [("ps", 0, 1)])
                self.A("act", T_["a"], pa, AF.Sigmoid, [("ps", 0, 1), "r_par"], [FK("a", hp)], bias=R["a0"][:, di, hp:hp + 1], scale=1.0)
                if rev:
                    self.MM(pa, R["lw"][64:128, 0, hp * 128:(hp + 1) * 128], al, True, True, ["r_lw", "r_pb"], [("ps", 0, 1)])
                    self.A("act", T_["a1"], pa, AF.Sigmoid, [("ps", 0, 1), "r_par"], [FK("a1", hp)], bias=R["a0"][:, 0, hp:hp + 1], scale=1.0)
                self.TS("dve", T_["kk"], k_, R["kk"][:, hp:hp + 1], None, ALU.mult, None, ["r_pb", "r_par"], [FK("kk", hp)])
                self.TT("pool", T_["tmp"], T_["kk"], T_["kk"], ALU.mult, [FK("kk", hp)], [FK("tmp", hp)])
                self.MM(pss, self.bd1, T_["tmp"], True, True, ["bd1", FK("tmp", hp)], [("ps", 1, 0)])
                self.A("act", T_["tmp2"], pss, AF.Sqrt, [("ps", 1, 0)], [FK("tmp2", hp)])
                self.TS("dve", T_["tmp2"], T_["tmp2"], 1e-12, None, ALU.max, None, [FK("tmp2", hp)], [FK("tmp2", hp)])
                S.op("dve", lambda o=T_["tmp2"]: nc.vector.reciprocal(out=o, in_=o), [FK("tmp2", hp)], [FK("tmp2", hp)])
                self.TT("dve", T_["kk"], T_["kk"], T_["tmp2"], ALU.mult, [FK("kk", hp), FK("tmp2", hp)], [FK("kk", hp)])
                self.TS("dve", T_["keff"], T_["a"], R["ka"][:, hp:hp + 1], R["omka"][:, hp:hp + 1], ALU.mult, ALU.add,
                        [FK("a", hp), "r_par", "r_omka"], [FK("keff", hp)])
                self.TT("pool", T_["keff"], T_["keff"], k_, ALU.mult, [FK("keff", hp), "r_pb"], [FK("keff", hp)])
                self.TT("pool", T_["beta"], T_["kk"], T_["a"], ALU.mult, [FK("kk", hp), FK("a", hp)], [FK("beta", hp)])
                for ch in range(nC):
                    src = v3(T_["sz"])[:, ch, :]; dst = v3(T_["cs"])[:, ch, :]
                    S.op("dve", lambda src=src, dst=dst: nc.vector.tensor_tensor_scan(
                        out=dst, data0=R["ones"][:, 0:64], data1=src, initial=0.0, op0=ALU.mult, op1=ALU.add),
                        [FK("sz", hp), "r_ones"], [FK("cs", hp)])
                cs3n = T_["cs"].rearrange("p (c t) -> p c t", t=64)
                csC = cs3n[:, :, 0:1] if rev else cs3n[:, :, 63:64]
                self.A("act", T_["E1"], T_["cs"], AF.Exp, [FK("cs", hp)], [FK("E1", hp)], scale=-CL)
                self.A("act", T_["Em"], T_["cs"], AF.Exp, [FK("cs", hp)], [FK("Em", hp)], scale=CL)
                self.TT("dve", T_["tmp"], T_["cs"], T_["sz"], ALU.subtract, [FK("cs", hp), FK("sz", hp)], [FK("tmp", hp)])
                self.A("act", T_["Ep"], T_["tmp"], AF.Exp, [FK("tmp", hp)], [FK("Ep", hp)], scale=-CL)
                csCb = bass.AP(csC.tensor, csC.offset, [list(csC.ap[0]), list(csC.ap[1]), [0, 64]])
                self.TT("dve", T_["tmp2"].rearrange("p (c t) -> p c t", t=64), csCb, cs3n, ALU.subtract, [FK("cs", hp)], [FK("tmp2", hp)])
                self.A("act", T_["Eh"], T_["tmp2"], AF.Exp, [FK("tmp2", hp)], [FK("Eh", hp)], scale=-CL)
                for hh in range(2):
                    h = 2 * hp + hh
                    rows = slice(64 * hh, 64 * hh + 64)
                    self.A("act", pc4[:, h, :], csC.rearrange("p c o -> p (c o)")[rows], AF.Exp, [FK("cs", hp)], [tg + "pc4"], scale=-CL)
                o3 = lambda nm: T_[nm].rearrange("p (c t) -> p c t", t=64)
                for hh in range(2):
                    h = 2 * hp + hh
                    rows = slice(64 * hh, 64 * hh + 64)
                    oh = lambda nm: opsT[nm][h].rearrange("p (c t) -> p c t", t=64)
                    S.op("dve", lambda oh=oh, rows=rows: nc.vector.scalar_tensor_tensor(out=oh("AT"), in0=v3(T_["kk"])[rows], scalar=-1.0, in1=v3(T_["Ep"])[rows],
                         op0=ALU.mult, op1=ALU.mult), [FK("kk", hp), FK("Ep", hp)], [OK_("AT", h)])
                    self.TT("pool", oh("BT"), v3(T_["beta"])[rows], v3(T_["Em"])[rows], ALU.mult, [FK("beta", hp), FK("Em", hp)], [OK_("BT", h)])
                    self.TT("dve", oh("KT"), v3(T_["keff"])[rows], v3(T_["Em"])[rows], ALU.mult, [FK("keff", hp), FK("Em", hp)], [OK_("KT", h)])
                    self.TT("pool", oh("RT"), v3(r_)[rows], v3(T_["E1"])[rows], ALU.mult, ["r_pb", FK("E1", hp)], [OK_("RT", h)])
                self.TT("dve", o3("BhT"), v3(T_["beta"]), v3(T_["Eh"]), ALU.mult, [FK("beta", hp), FK("Eh", hp)], [FK("BhT", hp)])
                self.TT("pool", o3("KhT"), v3(T_["keff"]), v3(T_["Eh"]), ALU.mult, [FK("keff", hp), FK("Eh", hp)], [FK("KhT", hp)])
                self.CP("act", o3("VT"), v3(v_), ["r_pb"], [FK("VT", hp)])
            if "rw_cut1" in self.dbg:
                return H
            for ch in range(nC):
                pt = self.ps[1]
                for j, nm in enumerate(("BhT", "KhT", "VT")):
                    for hp in range(2):
                        col = (j * 2 + hp) * 128
                        self.TR(pt[0:64, col:col + 128], fm[nm][hp][:, ch * 64:(ch + 1) * 64], self.ident,
                                ["ident", FK(nm, hp)], [("ps", 1, 0), ("ps", 1, 1)], inc=(j == 2 and hp == 1))
                self.CP("act", tok[:, ch].rearrange("p a b c -> p (a b c)"), pt[0:64, 0:768], [("ps", 1, 0), ("ps", 1, 1)], [tg + "tok"])
            if "rw_cut2" in self.dbg:
                return H
            def blk_ops(nm, hp, hh, ch):
                return opsT[nm][2 * hp + hh][:, ch * 64:(ch + 1) * 64]
            specs = (("NT", "AT", "BT", 0, 2), ("N", "BT", "AT", 1, 3), ("Aak", "KT", "AT", 1, 2), ("Gb", "BT", "RT", 2, 3), ("Gk", "KT", "RT", 2, 2))
            for (mn, ln, rn, mi, pi) in specs:
                pm = self.ps[pi][0:64, 0:NB * 64] if mn in ("NT", "Aak", "Gk") else self.ps[pi][0:64, 512:512 + NB * 64]
                pk = ("ps", pi, 0 if mn in ("NT", "Aak", "Gk") else 1)
                for ch in range(nC):
                    for h in range(4):
                        b_ = ch * 4 + h
                        hp, hh = h // 2, h % 2
                        self.MM(pm[:, b_ * 64:(b_ + 1) * 64], blk_ops(ln, hp, hh, ch), blk_ops(rn, hp, hh, ch), True, True,
                                [OK_(ln, h), OK_(rn, h)], [pk], inc=(b_ == NB - 1))
                mk = bass.AP(msk.tensor, msk[:, mi, :].offset, [list(msk.ap[0]), [0, NB], [1, 64]])
                self.TT("dve", mats[mn], pm.rearrange("p (b t) -> p b t", t=64), mk, ALU.mult, [pk, "r_msk"], [tg + mn])
            if "rw_cut3" in self.dbg:
                return H
            idb = bass.AP(ident64.tensor, ident64.offset, [list(ident64.ap[0]), [0, NB], [1, 64]])
            self.TT("dve", mats["Q"], mats["N"], idb, ALU.add, [tg + "N", "ident"], [tg + "Q"])
            cur = ("N", "NT", "Q"); nxt = ("N2", "NT2", "Q2")
            for kk_ in range(1, 6):
                pN = self.ps[2][0:64, 0:NB * 64]; pNT = self.ps[2][0:64, 512:512 + NB * 64]; pQ = self.ps[3][0:64, 0:NB * 64]
                last = kk_ == 5
                for b_ in range(NB):
                    if not last:
                        self.MM(pN[:, b_ * 64:(b_ + 1) * 64], mats[cur[1]][:, b_, :], mats[cur[0]][:, b_, :], True, True,
                                [tg + cur[0], tg + cur[1]], [("ps", 2, 0)], inc=(b_ == NB - 1))
                for b_ in range(NB):
                    self.MM(pNT[:, b_ * 64:(b_ + 1) * 64], mats[cur[0]][:, b_, :], mats[cur[1]][:, b_, :], True, True,
                            [tg + cur[0], tg + cur[1]], [("ps", 2, 1)], inc=(b_ == NB - 1))
                if not last:
                    self.CP("act", mats[nxt[0]], pN.rearrange("p (b t) -> p b t", t=64), [("ps", 2, 0)], [tg + nxt[0]])
                self.CP("dve", mats[nxt[1]], pNT.rearrange("p (b t) -> p b t", t=64), [("ps", 2, 1)], [tg + nxt[1]])
                for b_ in range(NB):
                    self.MM(pQ[:, b_ * 64:(b_ + 1) * 64], mats[nxt[1]][:, b_, :], mats[cur[2]][:, b_, :], True, True,
                            [tg + nxt[1], tg + cur[2]], [("ps", 3, 0)], inc=(b_ == NB - 1))
                self.TT("dve", mats[nxt[2]], mats[cur[2]], pQ.rearrange("p (b t) -> p b t", t=64), ALU.add, [tg + cur[2], ("ps", 3, 0)], [tg + nxt[2]])
                cur, nxt = nxt, cur
            Qn = cur[2]
            if "rw_cut4" in self.dbg:
                return H
            chs = list(range(nC))[::-1] if rev else list(range(nC))
            for ch in chs:
                pX = self.ps[3][0:64, 512:768]; pU = self.ps[3][0:64, 768:1024]
                pY = self.ps[0][0:64, 0:256]; pH = self.ps[0][0:64, 512:768]
                def Vtok(h):
                    return tok[:, ch, 2, h // 2, 64 * (h % 2):64 * (h % 2) + 64]
                for h in range(4):
                    hp, hh = h // 2, h % 2
                    b_ = ch * 4 + h
                    Hh = H[:, h, :]
                    self.MM(pX[:, h * 64:(h + 1) * 64], blk_ops("AT", hp, hh, ch), Hh, True, False, [OK_("AT", h), tg + "H"], [("ps", 3, 1)], inc=False)
                    self.MM(pX[:, h * 64:(h + 1) * 64], mats["Aak"][:, b_, :], Vtok(h), False, True, [tg + "Aak", tg + "tok"], [("ps", 3, 1)], inc=(h == 3))
                self.CP("act", Xs, pX, [("ps", 3, 1)], [tg + "Xs"])
                for h in range(4):
                    b_ = ch * 4 + h
                    self.MM(pU[:, h * 64:(h + 1) * 64], mats[Qn][:, b_, :], Xs[:, h * 64:(h + 1) * 64], True, True, [tg + Qn, tg + "Xs"], [("ps", 3, 1)], inc=(h == 3))
                self.CP("dve", Us, pU, [("ps", 3, 1)], [tg + "Us"])
                for h in range(4):
                    hp, hh = h // 2, h % 2
                    b_ = ch * 4 + h
                    Hh = H[:, h, :]
                    ysl = pY[:, h * 64:(h + 1) * 64]
                    self.MM(ysl, blk_ops("RT", hp, hh, ch), Hh, True, False, [OK_("RT", h), tg + "H"], [("ps", 0, 0)], inc=False)
                    self.MM(ysl, mats["Gb"][:, b_, :], Us[:, h * 64:(h + 1) * 64], False, False, [tg + "Gb", tg + "Us"], [("ps", 0, 0)], inc=False)
                    self.MM(ysl, mats["Gk"][:, b_, :], Vtok(h), False, True, [tg + "Gk", tg + "tok"], [("ps", 0, 0)], inc=(h == 3))
                for h in range(4):
                    hp, hh = h // 2, h % 2
                    hsl = pH[:, h * 64:(h + 1) * 64]
                    self.MM(hsl, tok[:, ch, 0, hp, 64 * hh:64 * hh + 64], Us[:, h * 64:(h + 1) * 64], True, False, [tg + "tok", tg + "Us"], [("ps", 0, 1)], inc=False)
                    self.MM(hsl, tok[:, ch, 1, hp, 64 * hh:64 * hh + 64], Vtok(h), False, True, [tg + "tok"], [("ps", 0, 1)], inc=(h == 3))
                self.CP("act", Ys, pY, [("ps", 0, 0)], [tg + "Ys"])
                for h in range(4):
                    self.STT(H[:, h, :], H[:, h, :], pc4[:, h, ch:ch + 1], pH[:, h * 64:(h + 1) * 64], ALU.mult, ALU.add,
                             [tg + "H", tg + "pc4", ("ps", 0, 1)], [tg + "H"])
                pT = self.ps[1][:, 512:640]
                for hp in range(2):
                    if rev:
                        self.MM(pT[:, hp * 64:(hp + 1) * 64], Ys[:, hp * 128:(hp + 1) * 128], R["J"], True, True, [tg + "Ys", "r_J"], [("ps", 1, 1)], inc=(hp == 1))
                    else:
                        self.TR(pT[:, hp * 64:(hp + 1) * 64], Ys[:, hp * 128:(hp + 1) * 128], ident64, [tg + "Ys", "ident"], [("ps", 1, 1)], inc=(hp == 1))
                ydst = y2[:, :, ch * 64:(ch + 1) * 64] if rev else y1[:, :, n0 + ch * 64:n0 + (ch + 1) * 64]
                self.CP("dve", ydst, pT.rearrange("p (a t) -> p a t", t=64), [("ps", 1, 1)], [tg + "y2" if rev else "r_y1"])
            if "rw_cut5" in self.dbg:
                return H
            if not rev:
                continue
            for hp in range(2):
                T_ = {nm: fm[nm][hp] for nm in names}
                r_ = pb[:, 0 + hp, n0:n0 + TB]; k_ = pb[:, 2 + hp, n0:n0 + TB]; v_ = pb[:, 4 + hp, n0:n0 + TB]
                p1 = self.ps[2][:, 0:TB]; p2 = self.ps[2][:, 512:512 + TB]; p3 = self.ps[3][:, 0:TB]; p4 = self.ps[1][:, 0:TB]
                self.TT("dve", T_["ysum"], y2[:, hp, :], y1[:, hp, n0:n0 + TB], ALU.add, [tg + "y2", "r_y1"], [FK("ysum", hp)])
                self.MM(p1, self.bd1, T_["ysum"], True, True, ["bd1", FK("ysum", hp)], [("ps", 2, 0)])
                self.STT(T_["yc"], p1, -1.0 / 64, T_["ysum"], ALU.mult, ALU.add, [("ps", 2, 0), FK("ysum", hp)], [FK("yc", hp)])
                self.TT("pool", T_["tmp"], T_["yc"], T_["yc"], ALU.mult, [FK("yc", hp)], [FK("tmp", hp)])
                self.MM(p2, self.bd1, T_["tmp"], True, True, ["bd1", FK("tmp", hp)], [("ps", 2, 1)])
                self.A("act", T_["tmp2"], p2, AF.Sqrt, [("ps", 2, 1), "r_eps"], [FK("tmp2", hp)], scale=1.0 / 64, bias=R["eps"][:, 0:1])
                S.op("dve", lambda o=T_["tmp2"]: nc.vector.reciprocal(out=o, in_=o), [FK("tmp2", hp)], [FK("tmp2", hp)])
                self.TT("dve", T_["yc"], T_["yc"], T_["tmp2"], ALU.mult, [FK("yc", hp), FK("tmp2", hp)], [FK("yc", hp)])
                self.TS("dve", T_["yc"], T_["yc"], R["lng"][:, hp:hp + 1], R["lnb"][:, hp:hp + 1], ALU.mult, ALU.add, [FK("yc", hp), "r_par"], [FK("yc", hp)])
                self.TT("pool", T_["tmp"], T_["a"], T_["a1"], ALU.add, [FK("a", hp), FK("a1", hp)], [FK("tmp", hp)])
                self.TS("dve", T_["tmp"], T_["tmp"], R["ka"][:, hp:hp + 1], R["omka2"][:, hp:hp + 1], ALU.mult, ALU.add, [FK("tmp", hp), "r_par", "r_omka2"], [FK("tmp", hp)])
                self.TT("pool", T_["tmp2"], r_, k_, ALU.mult, ["r_pb"], [FK("tmp2", hp)])
                self.STT(T_["tmp"], T_["tmp2"], R["rk"][:, hp:hp + 1], T_["tmp"], ALU.mult, ALU.mult, [FK("tmp2", hp), FK("tmp", hp), "r_par"], [FK("tmp", hp)])
                self.MM(p3, self.bd1, T_["tmp"], True, True, ["bd1", FK("tmp", hp)], [("ps", 3, 0)])
                self.TT("dve", T_["tmp2"], p3, v_, ALU.mult, [("ps", 3, 0), "r_pb"], [FK("tmp2", hp)])
                self.TT("pool", T_["yc"], T_["yc"], T_["tmp2"], ALU.add, [FK("yc", hp), FK("tmp2", hp)], [FK("yc", hp)])
                self.MM(p4, R["g2"][:, hp * 128:(hp + 1) * 128], sg, True, True, ["r_g2", "r_pb"], [("ps", 1, 0)])
                self.TT("dve", mixT[:, 2 + hp, n0:n0 + TB], T_["yc"], p4, ALU.mult, [FK("yc", hp), ("ps", 1, 0)], ["mixT"])
        return H

    def rwkv_stage(self, hT, mixT, phase):
        S = self.S
        with contextlib.ExitStack() as st:
            self.rwkv_setup(st)
            pb = self.sb(st, "r_pb", [128, 8, NTOK], BF16)
            with contextlib.ExitStack() as st2:
                self.rwkv_stage0(st2, hT, pb)
                S.barrier()
            y1 = self.sb(st, "r_y1", [128, 2, NTOK])
            if "rw_cut0" in self.dbg:
                S.barrier()
                return
            with contextlib.ExitStack() as st2:
                H = self.rwkv_pass(st2, 0, pb, y1, mixT)
                if phase == "A":
                    o = self.dout("rwkv_H_out", [64, 4, 64])
                    S.dma("sp", o, H, reads=["rp0_H"])
                S.barrier()
                if self.fused:
                    self.xchg(f"rwkv{self.layer}", H.rearrange("p a b -> p (a b)"), self.R["Hin"].rearrange("p a b -> p (a b)"), 64, 256, "rp0_H", "r_Hin")
            if phase != "A":
                with contextlib.ExitStack() as st2:
                    self.rwkv_pass(st2, 1, pb, y1, mixT)
                    S.barrier()
            S.barrier()

    def na_stage(self, hT, mixT):
        nc, S, I = self.nc, self.S, self.ins
        with contextlib.ExitStack() as st:
            sb = lambda n, sh, dt=F32: self.sb(st, n, sh, dt)
            wq = sb("n_wq", [128, 8, 128], BF16); wk = sb("n_wk", [128, 8, 128], BF16); wv = sb("n_wv", [128, 8, 128], BF16)
            qT = sb("n_qT", [128, NTOK], BF16)
            kT = sb("n_kT", [128, NEXT], BF16)
            V = sb("n_V", [128, 20, 2, 65], BF16)
            E = sb("n_E", [128, 3, 2, 640], BF16)
            stg = [sb(f"n_stg{i}", [128, 640]) for i in range(2)]
            Pb = [sb(f"n_P{i}", [128, 896], BF16) for i in range(2)]
            ysb = [sb(f"n_y{i}", [128, 128]) for i in range(2)]
            rc = [sb(f"n_rc{i}", [128, 2]) for i in range(2)]
            self.MS("pool", V[:, :, :, 64:65], 1.0, ["n_V1"])
            ip = 0
            for hp in range(4):
                self.load_w_bf16(wq, 1472 + 128 * hp, 128, "n_wq")
                self.load_w_bf16(wk, 1984 + 128 * hp, 128, "n_wk")
                self.load_w_bf16(wv, 2496 + 128 * hp, 128, "n_wv")
                for (h0, T, q0) in [(CTX0, 256, 0)] + [(LAT0 + i * 512, 512, 256 + i * 512) for i in range(4)]:
                    ps = self.ps[3]; pk = ("ps", 3, 0)
                    self.proj(ps, pk, wq, "n_wq", 0, 128, hT, h0, T)
                    self.A("act", qT[:, q0:q0 + T], ps[:, 0:T], AF.Copy, [pk], ["n_qT"], scale=0.125)
                for (h0, T, k0) in [(LAT0 + i * 512, 512, i * 512) for i in range(4)] + [(HAL0, 256, 2048), (CTX0, 256, 2304)]:
                    ps = self.ps[3][:, 512:1024]; pk = ("ps", 3, 1)
                    self.proj(ps, pk, wk, "n_wk", 0, 128, hT, h0, T)
                    self.CP("dve", kT[:, k0:k0 + T], ps[:, 0:T], [pk], ["n_kT"])
                for tt in range(20):
                    h0 = LAT0 + 128 * tt if tt < 16 else (HAL0 + 128 * (tt - 16) if tt < 18 else CTX0 + 128 * (tt - 18))
                    ps = self.ps[2][:, 512:640]; pk = ("ps", 2, 1)
                    for kc in range(8):
                        self.MM(ps, hT[:, kc, h0:h0 + 128], wv[:, kc, :], kc == 0, kc == 7, ["hT", "n_wv"], [pk])
                    self.CP("act" if tt % 2 else "dve", V[:, tt, :, 0:64], ps.rearrange("p (a d) -> p a d", d=64), [pk], ["n_V"])
                for cls in range(3):
                    for hh in range(2):
                        k = ip % 2; ip += 1
                        S.dma("sp", stg[k].rearrange("p (j q) -> p j q", q=128),
                              I["na_tab"][cls, 2 * hp + hh].rearrange("(j p) q -> p j q", p=128), writes=[("n_stg", k)])
                        self.A("act", E[:, cls, hh, :], stg[k], AF.Exp, [("n_stg", k)], ["n_E"])
                units = ([] if self.skip_ctx_out else [("c", 0), ("c", 1)]) + [("l", rp) for rp in range(16)]
                for ui, (kind, rp) in enumerate(units):
                    yk = ui % 2
                    psO = self.ps[2][:, 0:130]; pko = ("ps", 2, 0)
                    if kind == "l":
                        cls = min(rp, 2); base = max(rp - 2, 0)
                        kcols = [(base + j) * 128 for j in range(5)] + [2304, 2432]
                        vt = [base + j for j in range(5)] + [18, 19]
                        qc = 256 + rp * 128
                    else:
                        kcols = [2304, 2432]; vt = [18, 19]; qc = rp * 128
                    nk = len(kcols)
                    for hh in range(2):
                        psS = self.ps[hh]
                        pks = [("ps", hh, 0), ("ps", hh, 1)]
                        hs = slice(64 * hh, 64 * hh + 64)
                        for j, kc0 in enumerate(kcols):
                            self.MM(psS[:, j * 128:(j + 1) * 128], kT[hs, kc0:kc0 + 128], qT[hs, qc:qc + 128], True, True,
                                    ["n_kT", "n_qT"], [pks[j // 4]], inc=(j == nk - 1 or j == 3))
                    for hh in range(2):
                        psS = self.ps[hh]
                        pks = [("ps", hh, 0), ("ps", hh, 1)]
                        P = Pb[hh]
                        self.A("act", P[:, 0:nk * 128], psS[:, 0:nk * 128], AF.Exp, pks[0:(2 if nk > 4 else 1)], [("n_P", hh)])
                        if kind == "l":
                            self.TT("dve", P[:, 0:640], P[:, 0:640], E[:, cls, hh, :], ALU.mult, [("n_P", hh), "n_E"], [("n_P", hh)])
                    for hh in range(2):
                        P = Pb[hh]
                        for j in range(nk):
                            self.MM(psO[:, hh * 65:(hh + 1) * 65], P[:, j * 128:(j + 1) * 128], V[:, vt[j], hh, :], j == 0, j == nk - 1,
                                    [("n_P", hh), "n_V", "n_V1"], [pko], inc=(j == nk - 1 and hh == 1))
                    o3 = psO.rearrange("p (a d) -> p a d", d=65)
                    S.op("dve", lambda yk=yk, o3=o3: nc.vector.reciprocal(out=rc[yk].rearrange("p (a o) -> p a o", o=1), in_=o3[:, :, 64:65]), [pko], [("n_rc", yk)])
                    for hh in range(2):
                        self.TS("dve", ysb[yk][:, hh * 64:(hh + 1) * 64], psO[:, hh * 65:hh * 65 + 64], rc[yk][:, hh:hh + 1], None, ALU.mult, None,
                                [pko, ("n_rc", yk)], [("n_y", yk)])
                    pT = self.ps[3][:, 0:128] if yk == 0 else self.ps[3][:, 512:640]
                    pkt = ("ps", 3, yk)
                    self.TR(pT, ysb[yk], self.ident, [("n_y", yk), "ident"], [pkt])
                    mc = 256 + rp * 128 if kind == "l" else rp * 128
                    self.CP("act", mixT[:, 4 + hp, mc:mc + 128], pT, [pkt], ["mixT"])
            S.barrier()

    TOK_TILES = [(0, 256, 1)] + [(256 + i * 512, 512, 0) for i in range(4)]

    def wout_stage(self, xT, mixT):
        S, I = self.S, self.ins
        with contextlib.ExitStack() as st:
            wo = self.sb(st, "wo", [128, 8, D], BF16)
            S.dma("pool", wo, I["w_out"].rearrange("(c p) n -> p c n", p=128), writes=["wo"])
            xv = self.x_src.rearrange("(c p) n -> p c n", p=128)
            for c in range(8):
                S.dma("sp", xT[:, c, :], xv[:, c, 0:NTOK], writes=["xT"])
            i = 0
            for (c0, T, j) in self.tok_tiles:
                for dc in range(8):
                    ps = self.ps[i % 4][:, 0:T]; pk = ("ps", i % 4, 0)
                    i += 1
                    for fc in range(8):
                        self.MM(ps, wo[:, fc, dc * 128:(dc + 1) * 128], mixT[:, fc, c0:c0 + T], fc == 0, fc == 7, ["wo", "mixT"], [pk])
                    self.STT(xT[:, dc, c0:c0 + T], ps, self.mod[:, 16 + dc, j:j + 1], xT[:, dc, c0:c0 + T], ALU.mult, ALU.add,
                             [pk, "mod", "xT"], ["xT"])
            S.barrier()

    def norm2_router(self, st, xT, h2T, gT):
        nc, S, I = self.nc, self.S, self.ins
        with contextlib.ExitStack() as st2:
            sb = lambda n, sh, dt=F32: self.sb(st2, n, sh, dt)
            self.nsq = [sb(f"m_nsq{i}", [128, 512]) for i in range(2)]
            self.ntmp = [sb(f"m_ntmp{i}", [128, 512]) for i in range(2)]
            self.nrstd = sb("m_nrstd", [128, 512])
            h2f = sb("m_h2f", [128, 8, 512])
            rw = sb("m_rw", [128, 8, 32])
            S.dma("sp", rw, I["router_w"].rearrange("(c p) e -> p c e", p=128), writes=["m_rw"])
            rb = sb("m_rb", [128, 32])
            src = I["router_b"]
            S.dma("sp", rb, bass.AP(src.tensor, src.offset, [[0, 128], [1, 32]]), writes=["m_rb"])
            lg = sb("m_lg", [128, 32]); t8 = sb("m_t8", [128, 8]); mk = sb("m_mk", [128, 32]); ex = sb("m_ex", [128, 32])
            nm = sb("m_nm", [128, 1]); ss = sb("m_ss", [128, 1])
            for (c0, T, j) in self.tok_tiles:
                def out_fn(c, tmp, tk, c0=c0, T=T, j=j):
                    self.A("act", h2f[:, c, 0:T], tmp, AF.Identity, [tk, "gs2", "mod"], ["m_h2f"],
                           scale=self.gs2[:, c, j:j + 1], bias=self.mod[:, 24 + c, j:j + 1])
                    self.CP("pool" if c % 2 else "dve", h2T[:, c, c0:c0 + T], h2f[:, c, 0:T], ["m_h2f"], ["h2T"])
                self.norm_tile(xT[:, :, c0:c0 + T], "xT", T, self.gs2, 3, j, out_fn, "n2")
                for sub in range(T // 128):
                    pl = self.ps[2][:, 0:32]; pk = ("ps", 2, 0)
                    for c in range(8):
                        self.MM(pl, h2f[:, c, sub * 128:(sub + 1) * 128], rw[:, c, :], c == 0, c == 7, ["m_h2f", "m_rw"], [pk])
                    self.TT("dve", lg, pl, rb, ALU.add, [pk, "m_rb"], ["m_lg"])
                    S.op("dve", lambda: nc.vector.max(out=t8, in_=lg), ["m_lg"], ["m_t8"])
                    self.TS("dve", mk, lg, t8[:, 3:4], None, ALU.is_ge, None, ["m_lg", "m_t8"], ["m_mk"])
                    self.TS("dve", nm, t8[:, 0:1], -1.0, None, ALU.mult, None, ["m_t8"], ["m_nm"])
                    self.A("act", ex, lg, AF.Exp, ["m_lg", "m_nm"], ["m_ex"], bias=nm[:, 0:1], scale=1.0)
                    self.TT("dve", ex, ex, mk, ALU.mult, ["m_ex", "m_mk"], ["m_ex"])
                    S.op("dve", lambda: nc.vector.reduce_sum(out=ss, in_=ex, axis=AX.X), ["m_ex"], ["m_ss"])
                    S.op("dve", lambda: nc.vector.reciprocal(out=ss, in_=ss), ["m_ss"], ["m_ss"])
                    self.TS("dve", ex, ex, ss[:, 0:1], None, ALU.mult, None, ["m_ex", "m_ss"], ["m_ex"])
                    pt = self.ps[2][0:32, 512:640]; pkt = ("ps", 2, 1)
                    self.TR(pt, ex, self.ident, ["m_ex", "ident"], [pkt])
                    self.CP("act", gT[0:32, c0 + sub * 128:c0 + (sub + 1) * 128], pt, [pkt], ["gT"])
            S.barrier()

    def moe_stage(self, st, xT, h2T, gT):
        nc, S, I = self.nc, self.S, self.ins
        with contextlib.ExitStack() as st2:
            sb = lambda n, sh, dt=F32: self.sb(st2, n, sh, dt)
            ones32 = sb("e_ones", [32, 128])
            self.MS("pool", ones32, 1.0, ["e_ones"])
            gsel = sb("e_gsel", [32, 512])
            bgu = sb("e_bgu", [128, 32, 16])
            S.dma("sp", bgu, I["moe_b_gu"], writes=["e_bgu"])
            bdn = sb("e_bdn", [32, D])
            S.dma("sp", bdn, I["moe_b_dn"], writes=["e_bdn"])
            Gs = sb("e_Gs", [128, NTOK])
            wg = [sb(f"e_wg{i}", [128, 8, 512], BF16) for i in range(2)]
            wu = [sb(f"e_wu{i}", [128, 8, 512], BF16) for i in range(2)]
            wd = [sb(f"e_wd{i}", [128, 4, D], BF16) for i in range(2)]
            nb = 2
            gt = [sb(f"e_gt{i}", [128, 512]) for i in range(nb)]
            sg = [sb(f"e_sg{i}", [128, 512]) for i in range(nb)]
            ut = [sb(f"e_ut{i}", [128, 512]) for i in range(nb)]
            act = [sb(f"e_act{i}", [128, 4, 512], BF16) for i in range(2)]
            i = 0
            for (c0, T, j) in self.tok_tiles:
                for dc in range(8):
                    ps = self.ps[i % 4][:, 0:T]; pk = ("ps", i % 4, 0); i += 1
                    self.MM(ps, bdn[:, dc * 128:(dc + 1) * 128], gT[0:32, c0:c0 + T], True, True, ["e_bdn", "gT"], [pk])
                    self.STT(xT[:, dc, c0:c0 + T], ps, self.mod[:, 40 + dc, j:j + 1], xT[:, dc, c0:c0 + T], ALU.mult, ALU.add,
                             [pk, "mod", "xT"], ["xT"])
            wgu_v = I["moe_w_gu"]; wdn_v = I["moe_w_dn"]
            tiles = self.tok_tiles

            def load_piece(p):
                if p >= 64:
                    return
                e, half = p // 2, p % 2
                k = p % 2
                f0 = half * 512
                S.dma("pool", wg[k], wgu_v[e, :, f0:f0 + 512].rearrange("(c p) n -> p c n", p=128), writes=[("e_wg", k)])
                S.dma("pool", wu[k], wgu_v[e, :, D + f0:D + f0 + 512].rearrange("(c p) n -> p c n", p=128), writes=[("e_wu", k)])
                S.dma("pool", wd[k], wdn_v[e, f0:f0 + 512, :].rearrange("(c p) n -> p c n", p=128), writes=[("e_wd", k)])

            units = []
            for p in range(64):
                for ti, tl in enumerate(tiles):
                    units.append((p, ti, tl))
            cnt = {"it": 0}

            def emit_gu(n):
                p, ti, (c0, T, j) = units[n]
                e, half = p // 2, p % 2
                k = p % 2
                ak = n % 2
                if half == 0 and ti == 0:
                    for (c0g, Tg, jg) in tiles:
                        ps = self.ps[3][:, 512:512 + Tg]; pk = ("ps", 3, 1)
                        self.TS("dve", gsel[:, 0:Tg], gT[0:32, c0g:c0g + Tg], self.ident[0:32, e:e + 1], None, ALU.mult, None, ["gT", "ident"], ["e_gsel"])
                        self.MM(ps, ones32, gsel[:, 0:Tg], True, True, ["e_ones", "e_gsel"], [pk])
                        self.CP("act", Gs[:, c0g:c0g + Tg], ps, [pk], ["e_Gs"])
                for fci in range(4):
                    b_ = cnt["it"] % nb; cnt["it"] += 1
                    psg = self.ps[0][:, 0:T] if fci % 2 == 0 else self.ps[0][:, 512:512 + T]
                    psu = self.ps[1][:, 0:T] if fci % 2 == 0 else self.ps[1][:, 512:512 + T]
                    pkg = ("ps", 0, fci % 2); pku = ("ps", 1, fci % 2)
                    for kc in range(8):
                        self.MM(psg, wg[k][:, kc, fci * 128:(fci + 1) * 128], h2T[:, kc, c0:c0 + T], kc == 0, kc == 7, [("e_wg", k), "h2T"], [pkg])
                    for kc in range(8):
                        self.MM(psu, wu[k][:, kc, fci * 128:(fci + 1) * 128], h2T[:, kc, c0:c0 + T], kc == 0, kc == 7, [("e_wu", k), "h2T"], [pku])
                    fc16 = half * 4 + fci
                    g_ = gt[b_][:, 0:T]; s_ = sg[b_][:, 0:T]; u_ = ut[b_][:, 0:T]
                    self.TS("dve", g_, psg, bgu[:, e, fc16:fc16 + 1], 7.0, ALU.add, ALU.min, [pkg, "e_bgu"], [("e_gt", b_)])
                    self.A("act", s_, g_, AF.Sigmoid, [("e_gt", b_)], [("e_sg", b_)], scale=1.702)
                    self.TS("dve", u_, psu, bgu[:, e, 8 + fc16:8 + fc16 + 1], 7.0, ALU.add, ALU.min, [pku, "e_bgu"], [("e_ut", b_)])
                    self.TS("dve", u_, u_, -7.0, 1.0, ALU.max, ALU.add, [("e_ut", b_)], [("e_ut", b_)])
                    self.TT("dve", g_, g_, s_, ALU.mult, [("e_gt", b_), ("e_sg", b_)], [("e_gt", b_)])
                    self.TT("pool", u_, u_, Gs[:, c0:c0 + T], ALU.mult, [("e_ut", b_), "e_Gs"], [("e_ut", b_)])
                    self.TT("pool", act[ak][:, fci, 0:T], u_, g_, ALU.mult, [("e_ut", b_), ("e_gt", b_)], [("e_act", ak)])

            def emit_dn(n):
                p, ti, (c0, T, j) = units[n]
                k = p % 2
                ak = n % 2
                for dc in range(8):
                    pso = self.ps[2][:, 0:T] if dc % 2 == 0 else self.ps[2][:, 512:512 + T]
                    pko = ("ps", 2, dc % 2)
                    for fci in range(4):
                        self.MM(pso, wd[k][:, fci, dc * 128:(dc + 1) * 128], act[ak][:, fci, 0:T], fci == 0, fci == 3, [("e_wd", k), ("e_act", ak)], [pko])
                    self.STT(xT[:, dc, c0:c0 + T], pso, self.mod[:, 40 + dc, j:j + 1], xT[:, dc, c0:c0 + T], ALU.mult, ALU.add,
                             [pko, "mod", ("xT", dc)], [("xT", dc)])
                if ti == len(tiles) - 1:
                    load_piece(p + 2)

            load_piece(0); load_piece(1)
            emit_gu(0)
            for n in range(len(units)):
                if n + 1 < len(units):
                    emit_gu(n + 1)
                emit_dn(n)
            S.barrier()

    def final_stage(self, xT, last):
        nc, S, I = self.nc, self.S, self.ins
        with contextlib.ExitStack() as st2:
            sb = lambda n, sh, dt=F32: self.sb(st2, n, sh, dt)
            if not last:
                o = self.dout("xT_out", [D, NTOK])
                ov = o.rearrange("(c p) n -> p c n", p=128)
                for c in range(8):
                    S.dma("sp", ov[:, c, :], xT[:, c, :], reads=["xT"])
            else:
                self.nsq = [sb(f"f_nsq{i}", [128, 512]) for i in range(2)]
                self.ntmp = [sb(f"f_ntmp{i}", [128, 512]) for i in range(2)]
                self.nrstd = sb("f_nrstd", [128, 512])
                ob = [sb(f"f_ob{i}", [128, 8, 512]) for i in range(2)]
                o = self.dout("outT", [D, OWN])
                ov = o.rearrange("(c p) n -> p c n", p=128)
                for ti in range(4):
                    c0 = 256 + ti * 512
                    obt = ob[ti % 2]
                    def out_fn(c, tmp, tk, obt=obt, ti=ti):
                        self.TS("dve", obt[:, c, :], tmp, self.gfin[:, c:c + 1], None, ALU.mult, None, [tk, "gfin"], [("f_ob", ti % 2)])
                    self.norm_tile(xT[:, :, c0:c0 + 512], "xT", 512, None, None, 0, out_fn, "nf")
                    S.dma("sp", ov[:, :, ti * 512:(ti + 1) * 512], obt, reads=[("f_ob", ti % 2)])
            S.barrier()

    def handoff_stage(self, xT, xs1):
        S = self.S
        with contextlib.ExitStack() as st2:
            xv = xs1.rearrange("(c p) n -> p c n", p=128)
            for c in range(8):
                S.dma("sp", xv[:, c, 0:NTOK], xT[:, c, :], reads=["xT"], writes=["xs1"])
            blk = self.sb(st2, "ho_blk", [128, 8, 256]); got = self.sb(st2, "ho_got", [128, 8, 256]); rev = self.sb(st2, "ho_rev", [128, 8, 256])
            self.CP("dve", blk, xT[:, :, NTOK - 256:NTOK], ["xT"], ["ho_blk"])
            self.xchg("halo", blk.rearrange("p a b -> p (a b)"), got.rearrange("p a b -> p (a b)"), 128, 2048, "ho_blk", "ho_got")
            self.CP("dve", rev, got[:, :, ::-1], ["ho_got"], ["ho_rev"])
            for c in range(8):
                S.dma("sp", xv[:, c, NTOK:NEXT], rev[:, c, :], reads=["ho_rev"], writes=["xs1"])
            S.barrier()


def build_program(phase="B", last=False, dbg=(), stop_after=None):
    P = Prog(dbg)
    P.skip_ctx_out = "skip_ctx_out" in P.dbg
    if P.skip_ctx_out:
        P.tok_tiles = [(256 + i * 512, 512, 0) for i in range(4)]
    with contextlib.ExitStack() as es:
        P.setup(es)
        P.x_src = P.ins["xT"]
        P.adaln()
        mixT = P.sb(es, "mixT", [128, 8, NTOK], BF16)

        def dump_bf(stk, name, src, chunks, key):
            tmpf = P.sb(stk, "dbg_" + name, [128, src.shape[-1]])
            for c in chunks:
                o = P.dout(f"dbg_{name}{c}", [128, src.shape[-1]])
                P.CP("dve", tmpf, src[:, c, :], [key], ["dbgf"])
                P.S.dma("sp", o, tmpf, reads=["dbgf"])
            P.S.barrier()

        with contextlib.ExitStack() as sh:
            hT = P.sb(sh, "hT", [128, 8, NTP], BF16)
            P.norm1(hT)
            if stop_after == "norm1":
                dump_bf(sh, "hT", hT, range(8), "hT")
                return P
            if "skip_lru" not in P.dbg:
                P.lru_stage(hT, mixT, phase)
            if stop_after == "lru":
                if phase == "B":
                    dump_bf(sh, "mix", mixT, (0, 1), "mixT")
                P.S.barrier()
                return P
            if "skip_rwkv" not in P.dbg:
                P.rwkv_stage(hT, mixT, phase)
            if stop_after == "rwkv":
                if phase == "B":
                    dump_bf(sh, "mix", mixT, (2, 3), "mixT")
                P.S.barrier()
                return P
            if phase == "A":
                P.S.barrier()
                return P
            if "skip_na" not in P.dbg:
                P.na_stage(hT, mixT)
            if stop_after == "na":
                dump_bf(sh, "mix", mixT, range(8), "mixT")
                return P
            P.S.barrier()
        with contextlib.ExitStack() as sx:
            xT = P.sb(sx, "xres", [128, 8, NTOK])
            P.wout_stage(xT, mixT)
            if stop_after == "wout":
                o = P.dout("dbg_xmid", [D, NTOK])
                ov = o.rearrange("(c p) n -> p c n", p=128)
                for c in range(8):
                    P.S.dma("sp", ov[:, c, :], xT[:, c, :], reads=["xT"])
                P.S.barrier()
                return P
            h2T = mixT
            gT = P.sb(sx, "gT", [32, NTOK])
            P.norm2_router(sx, xT, h2T, gT)
            if stop_after == "norm2":
                o = P.dout("dbg_gT", [32, NTOK])
                P.S.dma("sp", o, gT, reads=["gT"])
                dump_bf(sx, "h2", h2T, range(8), "h2T")
                return P
            P.moe_stage(sx, xT, h2T, gT)
            P.final_stage(xT, last)
            P.S.barrier()
    return P


def build_fused():
    P = Prog(())
    P.fused = True
    with contextlib.ExitStack() as es:
        P.setup(es)
        xs1 = P.nc.dram_tensor("x_scratch1", [D, NEXT], F32).ap()
        P.layer = 0
        mixT = P.sb(es, "mixT", [128, 8, NTOK], BF16)
        for l in range(2):
            P.layer = l
            P.x_src = P.ins["xT"] if l == 0 else xs1
            if l == 1:
                P.tok_tiles = [(256 + i * 512, 512, 0) for i in range(4)]
                P.skip_ctx_out = True
            P.adaln()
            with contextlib.ExitStack() as sh:
                hT = P.sb(sh, "hT", [128, 8, NTP], BF16)
                P.norm1(hT)
                P.lru_stage(hT, mixT, "F")
                P.rwkv_stage(hT, mixT, "F")
                P.na_stage(hT, mixT)
                P.S.barrier()
            with contextlib.ExitStack() as sx:
                xT = P.sb(sx, "xres", [128, 8, NTOK])
                P.wout_stage(xT, mixT)
                gT = P.sb(sx, "gT", [32, NTOK])
                P.norm2_router(sx, xT, mixT, gT)
                P.moe_stage(sx, xT, mixT, gT)
                if l == 0:
                    P.handoff_stage(xT, xs1)
                else:
                    P.final_stage(xT, True)
                P.S.barrier()
    return P


_NA_IDX = {}


def _na_index(s):
    if s in _NA_IDX:
        return _NA_IDX[s]
    key = np.arange(640)
    q = np.arange(128)
    br, kc_l = key // 64, key % 64
    qr, qc_l = q // 64, q % 64
    ri = np.zeros((3, 640, 128), np.int64)
    ci = np.zeros((3, 640, 128), np.int64)
    ok = np.zeros((3, 640, 128), bool)
    for cls in range(3):
        qrow_l = 2 * cls + qr
        krow_l = br
        if s == 0:
            r, c, kr, kc = qrow_l[None, :], qc_l[None, :], krow_l[:, None], kc_l[:, None]
        else:
            r, c, kr, kc = 63 - qrow_l[None, :], 63 - qc_l[None, :], 63 - krow_l[:, None], 63 - kc_l[:, None]
        rs = np.clip(r - 4, 0, 56)
        cs = np.clip(c - 8, 0, 48)
        v = (kr >= rs) & (kr < rs + 8) & (kc >= cs) & (kc < cs + 16)
        ok[cls] = v
        ri[cls] = np.where(v, kr - r + 7, 0)
        ci[cls] = np.where(v, kc - c + 15, 0)
    _NA_IDX[s] = (ri, ci, ok)
    return _NA_IDX[s]


_CONSTS = {}


def _consts():
    if not _CONSTS:
        _CONSTS["c_ident"] = np.eye(128, dtype=np.float32)
        sel = np.zeros((32, 32, 128), np.float32)
        for e in range(32):
            sel[e, e, :] = 1.0
        _CONSTS["c_sel"] = sel.reshape(32, 32 * 128)
        i = np.arange(64)
        lo = (i[None, :] < i[:, None])
        up = (i[:, None] < i[None, :])
        upi = (i[:, None] <= i[None, :])
        _CONSTS["c_masks"] = np.stack([lo, up, upi], 1).astype(np.float32)
    return _CONSTS


def local_order(xl_b, xc_b, s):
    if s == 0:
        return xc_b, xl_b[0:2048], xl_b[2048:2304]
    return xc_b[::-1], xl_b[4095:2047:-1], xl_b[2047:1791:-1]


def prep_core(inp, l, core, xl, xc):
    b, s = core // 2, core % 2
    d1, d2 = s, 1 - s
    m = {}
    ctx_, own_, halo_ = local_order(xl[b], xc[b], s)
    m["xT"] = np.ascontiguousarray(np.concatenate([ctx_, own_, halo_], 0).T)
    def pc(v):
        v = np.asarray(v)
        n = v.shape[-1] // 128
        v = v.reshape(v.shape[:-1] + (n, 128))
        return np.moveaxis(v, -1, 0)
    m["cvec"] = np.stack([pc(inp["c"][b]), pc(inp["c_ctx"])], -1)
    m["ada_w"] = inp["ada_w"][l]; m["ada_b"] = pc(inp["ada_b"][l])
    m["g_mix"] = pc(inp["norm_mix_g"][l]); m["g_ffn"] = pc(inp["norm_ffn_g"][l])
    m["w_in"] = inp["w_in"][l]; m["w_out"] = inp["w_out"][l]
    dd = [d1, d2]
    m["lru_cw"] = np.moveaxis(pc(inp["lru_conv_w"][l][dd]), 2, 3)
    m["lru_cb"] = pc(inp["lru_conv_b"][l][dd])
    m["lru_br"] = pc(inp["lru_br"][l][dd]); m["lru_bi"] = pc(inp["lru_bi"][l][dd]); m["lru_lam"] = pc(inp["lru_lambda"][l][dd])
    m["lru_wr"] = inp["lru_wr"][l][dd]; m["lru_wi"] = inp["lru_wi"][l][dd]
    m["rwkv_mu"] = inp["rwkv_mu"][l][[0, 1]] if s == 0 else inp["rwkv_mu"][l][[1, 0]]
    m["rwkv_w0"] = pc(inp["rwkv_w0"][l][dd]); m["rwkv_a0"] = pc(inp["rwkv_a0"][l][dd])
    m["rwkv_w2"] = inp["rwkv_w2"][l][dd]; m["rwkv_a2"] = inp["rwkv_a2"][l][dd]
    m["rwkv_g2"] = inp["rwkv_g2"][l]
    m["rwkv_kk"] = pc(inp["rwkv_kk"][l]); m["rwkv_ka"] = pc(inp["rwkv_ka"][l]); m["rwkv_rk"] = pc(inp["rwkv_rk"][l].reshape(256))
    m["rwkv_lng"] = pc(inp["rwkv_lnx_g"][l]); m["rwkv_lnb"] = pc(inp["rwkv_lnx_b"][l])
    ri, ci, ok = _na_index(s)
    rpb = inp["na_rpb"][l]
    tab = rpb[:, ri, ci]
    tab = np.where(ok[None], tab, np.float32(-30000.0))
    m["na_tab"] = np.ascontiguousarray(tab.transpose(1, 0, 2, 3)).astype(np.float32)
    m["router_w"] = inp["router_w"][l]; m["router_b"] = inp["router_b"][l].reshape(1, 32)
    m["moe_w_gu"] = inp["moe_w_gu"][l]; m["moe_b_gu"] = pc(inp["moe_b_gu"][l])
    m["moe_w_dn"] = inp["moe_w_dn"][l]; m["moe_b_dn"] = inp["moe_b_dn"][l]
    m["final_g"] = pc(inp["final_g"])
    m.update(_consts())
    m["lru_h_in"] = np.zeros((128, 2), np.float32)
    m["rwkv_H_in"] = np.zeros((64, 4, 64), np.float32)
    return {k: np.ascontiguousarray(v, dtype=np.float32) for k, v in m.items()}


_PROGS = {}


def _prog(phase, last):
    k = (phase, last)
    if k not in _PROGS:
        _PROGS[k] = build_program(phase=phase, last=last)
    return _PROGS[k]


def _launch(P, maps):
    res = run_bass_kernel_spmd(P.nc, [{k: m[k] for k in P.ins} for m in maps], core_ids=list(range(8)))
    return res.results


def kernel_unfused(**inputs):
    inp = {k: np.asarray(v, dtype=np.float32) for k, v in inputs.items()}
    xl = inp["x"]
    xc = inp["ctx"]
    out = None
    for l in range(2):
        last = l == 1
        maps = [prep_core(inp, l, c, xl, xc) for c in range(8)]
        ra = _launch(_prog("A", False), maps)
        for c in range(8):
            maps[c]["lru_h_in"] = ra[c ^ 1]["lru_h_out"]
            maps[c]["rwkv_H_in"] = ra[c ^ 1]["rwkv_H_out"]
        rb = _launch(_prog("B", last), maps)
        if not last:
            xl_n = np.empty_like(xl)
            xc_n = np.empty_like(xc)
            for c in range(8):
                b, s = c // 2, c % 2
                xo = rb[c]["xT_out"].T
                if s == 0:
                    xl_n[b, 0:2048] = xo[256:]
                    xc_n[b] = xo[:256]
                else:
                    xl_n[b, 2048:4096] = xo[256:][::-1]
            xl, xc = xl_n, xc_n
        else:
            out = np.empty((4, 4096, D), np.float32)
            for c in range(8):
                b, s = c // 2, c % 2
                yo = rb[c]["outT"].T
                if s == 0:
                    out[b, 0:2048] = yo
                else:
                    out[b, 2048:4096] = yo[::-1]
    return out


_FUSED = {}


def kernel(**inputs):
    inp = {k: np.asarray(v, dtype=np.float32) for k, v in inputs.items()}
    if "P" not in _FUSED:
        _FUSED["P"] = build_fused()
    P = _FUSED["P"]
    maps = []
    for c in range(8):
        s = c % 2
        m0 = prep_core(inp, 0, c, inp["x"], inp["ctx"])
        m1 = prep_core(inp, 1, c, inp["x"], inp["ctx"])
        m = {}
        for k, v in m0.items():
            if k in GLOBAL_INPUTS:
                m[k] = v
            else:
                m[k + "_L0"] = v
        for k, v in m1.items():
            if k not in GLOBAL_INPUTS:
                m[k + "_L1"] = v
        ohs = np.zeros((128, 2), np.float32); ohs[:, s] = 1.0
        ohp = np.zeros((128, 2), np.float32); ohp[:, 1 - s] = 1.0
        m["oh_self"] = ohs; m["oh_part"] = ohp
        maps.append({k: m[k] for k in P.ins})
    res = run_bass_kernel_spmd(P.nc, maps, core_ids=list(range(8))).results
    out = np.empty((4, 4096, D), np.float32)
    for c in range(8):
        b, s = c // 2, c % 2
        yo = res[c]["outT"].T
        if s == 0:
            out[b, 0:2048] = yo
        else:
            out[b, 2048:4096] = yo[::-1]
    return out
```

```python
import contextlib

import numpy as np
import concourse.bass as bass
import concourse.mybir as mybir
from concourse.bass_utils import run_bass_kernel_spmd

F32 = mybir.dt.float32
BF16 = mybir.dt.bfloat16
AF = mybir.ActivationFunctionType
ALU = mybir.AluOpType
AX = mybir.AxisListType

D = 1024
NCH = 8
CTX = 256
OWN = 2048
HALO = 256
NTOK = CTX + OWN
NEXT = CTX + OWN + HALO
CTX0 = 1
LAT0 = 258
HAL0 = LAT0 + OWN
NTP = HAL0 + HALO
IN_COLS = 3008
XC0 = 3
XL0 = 262
NXA = 2320
SEM_ROT = 30000


class Sched:
    def __init__(self, nc, es, n_dma_sems=12):
        self.nc = nc
        self.es = es
        self.engs = {"pe": nc.tensor, "act": nc.scalar, "dve": nc.vector,
                     "pool": nc.gpsimd, "sp": nc.sync}
        self.sem_id = 0
        self.cur_sem = {}
        self.cnt = {}
        for e in self.engs:
            self._new_sem(e)
        self.seen = {e: {} for e in self.engs}
        self.last_w = {}
        self.readers = {}
        self.dma_sems = {}
        self.dma_idx = {}
        for q in ("sp", "pool", "act"):
            self.dma_sems[q] = [self._alloc_sem(f"d{q}{i}") for i in range(n_dma_sems)]
            self.dma_idx[q] = 0
        self.dma_val = {}
        self.n_wait = 0
        self.n_ins = 0
        self.pending = {e: False for e in self.engs}

    def _alloc_sem(self, name):
        self.sem_id += 1
        return self.es.enter_context(self.nc.semaphore(f"{name}_{self.sem_id}"))

    def _new_sem(self, e):
        self.cur_sem[e] = self._alloc_sem("s" + e)
        self.cnt[e] = 0

    def _wait(self, e, ev):
        sem, val = ev
        k = id(sem)
        if self.seen[e].get(k, 0) >= val:
            return
        self.engs[e].wait_ge(sem, val)
        self.n_wait += 1
        self.seen[e][k] = val

    def _deps(self, reads, writes):
        evs = []
        for r in reads:
            w = self.last_w.get(r)
            if w is not None:
                evs.append(w)
        for w_ in writes:
            w = self.last_w.get(w_)
            if w is not None:
                evs.append(w)
            evs.extend(self.readers.get(w_, ()))
        return evs

    def _record(self, ev, reads, writes):
        for r in reads:
            self.readers.setdefault(r, []).append(ev)
        for w_ in writes:
            self.last_w[w_] = ev
            self.readers[w_] = []

    def op(self, e, fn, reads=(), writes=(), inc=True):
        own = self.cur_sem[e]
        for ev in self._deps(reads, writes):
            if ev[0] is own and e == "pe":
                continue
            if ev[0] is own and ev[1] > self.cnt[e]:
                continue
            self._wait(e, ev)
        ins = fn()
        self.n_ins += 1
        ev = (own, self.cnt[e] + 1)
        if inc:
            ins.then_inc(own, 1)
            self.cnt[e] += 1
            self.pending[e] = False
            if self.cnt[e] >= SEM_ROT:
                self._new_sem(e)
        else:
            self.pending[e] = True
        self._record(ev, reads, writes)
        return ins

    def dma(self, q, out, in_, reads=(), writes=(), **kw):
        for ev in self._deps(reads, writes):
            self._wait(q, ev)
        i = self.dma_idx[q]
        self.dma_idx[q] = (i + 1) % len(self.dma_sems[q])
        sem = self.dma_sems[q][i]
        prev = self.dma_val.get(id(sem), 0)
        if prev:
            self._wait(q, (sem, prev))
        ins = self.engs[q].dma_start(out=out, in_=in_, **kw)
        ins.then_inc(sem, 16)
        self.n_ins += 1
        val = prev + 16
        self.dma_val[id(sem)] = val
        ev = (sem, val)
        self._record(ev, reads, writes)
        return ev

    def flush(self, e):
        if self.pending[e]:
            own = self.cur_sem[e]
            self.engs[e].nop().then_inc(own, 1)
            self.cnt[e] += 1
            self.pending[e] = False

    def barrier(self):
        for e in self.engs:
            self.flush(e)
        evs = []
        for e in self.engs:
            if self.cnt[e] > 0:
                evs.append((self.cur_sem[e], self.cnt[e]))
        for q in self.dma_sems:
            for sem in self.dma_sems[q]:
                v = self.dma_val.get(id(sem), 0)
                if v:
                    evs.append((sem, v))
        for e in self.engs:
            for ev in evs:
                if ev[0] is self.cur_sem[e] and e == "pe":
                    continue
                self._wait(e, ev)
        self.last_w = {}
        self.readers = {}


INPUT_SHAPES = {
    "xT": [D, NEXT],
    "cvec": [128, 8, 2],
    "ada_w": [D, 6 * D],
    "ada_b": [128, 48],
    "g_mix": [128, 8],
    "g_ffn": [128, 8],
    "w_in": [D, IN_COLS],
    "w_out": [D, D],
    "lru_cw": [128, 2, 2, 4],
    "lru_cb": [128, 2, 2],
    "lru_br": [128, 2, 2],
    "lru_bi": [128, 2, 2],
    "lru_lam": [128, 2, 2],
    "lru_wr": [2, 4, 64, 64],
    "lru_wi": [2, 4, 64, 64],
    "rwkv_mu": [2, 960],
    "rwkv_w0": [128, 2, 2],
    "rwkv_a0": [128, 2, 2],
    "rwkv_w2": [2, 64, 256],
    "rwkv_a2": [2, 64, 256],
    "rwkv_g2": [64, 256],
    "rwkv_kk": [128, 2],
    "rwkv_ka": [128, 2],
    "rwkv_rk": [128, 2],
    "rwkv_lng": [128, 2],
    "rwkv_lnb": [128, 2],
    "na_tab": [3, 8, 640, 128],
    "router_w": [D, 32],
    "router_b": [1, 32],
    "moe_w_gu": [32, D, 2 * D],
    "moe_b_gu": [128, 32, 16],
    "moe_w_dn": [32, D, D],
    "moe_b_dn": [32, D],
    "final_g": [128, 8],
    "c_ident": [128, 128],
    "c_sel": [32, 32 * 128],
    "c_masks": [64, 3, 64],
    "oh_self": [128, 2],
    "oh_part": [128, 2],
    "lru_h_in": [128, 2],
    "rwkv_H_in": [64, 4, 64],
}


GLOBAL_INPUTS = ("xT", "cvec", "final_g", "c_ident", "c_sel", "c_masks", "lru_h_in", "rwkv_H_in", "oh_self", "oh_part")


class _LazyIns(dict):
    def __init__(self, prog):
        super().__init__()
        self.prog = prog

    def __getitem__(self, name):
        if self.prog.fused and name not in GLOBAL_INPUTS:
            real = f"{name}_L{self.prog.layer}"
        else:
            real = name
        if real not in self:
            ap = self.prog.nc.dram_tensor(real, list(INPUT_SHAPES[name]), F32, kind="ExternalInput").ap()
            dict.__setitem__(self, real, ap)
        return dict.__getitem__(self, real)


class Prog:
    def __init__(self, dbg=()):
        self.nc = bass.Bass("TRN2", target_bir_lowering=False)
        self.dbg = set(dbg)
        self.ins = _LazyIns(self)
        self.outs = {}
        self.fused = False
        self.layer = 0
        self.x_src = None
        self.skip_ctx_out = False
        self.tok_tiles = [(0, 256, 1)] + [(256 + i * 512, 512, 0) for i in range(4)]

    def din(self, name, shape, dt=F32):
        ap = self.nc.dram_tensor(name, list(shape), dt, kind="ExternalInput").ap()
        self.ins[name] = ap
        return ap

    def dout(self, name, shape, dt=F32):
        ap = self.nc.dram_tensor(name, list(shape), dt, kind="ExternalOutput").ap()
        self.outs[name] = ap
        return ap

    def sb(self, st, name, shape, dt=F32):
        return st.enter_context(self.nc.sbuf_tensor(f"{name}_l{self.layer}", list(shape), dt)).ap()

    def xchg(self, tag, src, dst, P_, F_, src_key, dst_key):
        nc, S = self.nc, self.S
        with contextlib.ExitStack() as st:
            ctr = self.sb(st, f"xc_{tag}_c", [P_, 2, F_]); g = self.sb(st, f"xc_{tag}_g", [P_, 2, F_])
            din_ = nc.dram_tensor(f"cc_in_{tag}", [P_, 2 * F_], F32)
            dou = nc.dram_tensor(f"cc_out_{tag}", [P_, 2 * F_], F32)
            ck, gk = ("xc_c", tag), ("xc_g", tag)
            for k in range(2):
                self.TS("dve", ctr[:, k, :], src, self.ohs[0:P_, k:k + 1], None, ALU.mult, None, [src_key, "ohs"], [ck])
            S.dma("pool", din_.ap(), ctr.rearrange("p a b -> p (a b)"), reads=[ck], writes=[("ccin", tag)])
            S.op("pool", lambda: nc.gpsimd.collective_compute("AllReduce", ALU.add, replica_groups=[[0, 1], [2, 3], [4, 5], [6, 7]],
                                                             ins=[din_.ap().opt()], outs=[dou.ap().opt()]),
                 [("ccin", tag)], [("ccout", tag)])
            S.dma("pool", g.rearrange("p a b -> p (a b)"), dou.ap(), reads=[("ccout", tag)], writes=[gk])
            self.TS("dve", dst, g[:, 0, :], self.ohp[0:P_, 0:1], None, ALU.mult, None, [gk, "ohp"], [dst_key])
            self.STT(dst, g[:, 1, :], self.ohp[0:P_, 1:2], dst, ALU.mult, ALU.add, [gk, "ohp", dst_key], [dst_key])
            S.barrier()

    def declare_inputs(self):
        pass

    def A(self, e, out, in_, func, reads, writes, **kw):
        nc = self.nc
        return self.S.op(e, lambda: nc.scalar.activation(out=out, in_=in_, func=func, **kw), reads, writes)

    def eng(self, e):
        return {"dve": self.nc.vector, "pool": self.nc.gpsimd}[e]

    def TT(self, e, out, in0, in1, op, reads, writes):
        en = self.eng(e)
        return self.S.op(e, lambda: en.tensor_tensor(out=out, in0=in0, in1=in1, op=op), reads, writes)

    def TS(self, e, out, in0, s1, s2, op0, op1, reads, writes):
        en = self.eng(e)
        if op1 is None:
            return self.S.op(e, lambda: en.tensor_scalar(out=out, in0=in0, scalar1=s1, scalar2=None, op0=op0), reads, writes)
        return self.S.op(e, lambda: en.tensor_scalar(out=out, in0=in0, scalar1=s1, scalar2=s2, op0=op0, op1=op1), reads, writes)

    def STT(self, out, in0, scalar, in1, op0, op1, reads, writes):
        nc = self.nc
        return self.S.op("dve", lambda: nc.vector.scalar_tensor_tensor(out=out, in0=in0, scalar=scalar, in1=in1, op0=op0, op1=op1), reads, writes)

    def MM(self, out, lhsT, rhs, start, stop, reads, writes, inc=None):
        nc = self.nc
        if inc is None:
            inc = stop
        return self.S.op("pe", lambda: nc.tensor.matmul(out, lhsT, rhs, start=start, stop=stop), reads, writes, inc=inc)

    def TR(self, out, in_, ident, reads, writes, inc=True):
        nc = self.nc
        return self.S.op("pe", lambda: nc.tensor.transpose(out, in_, ident), reads, writes, inc=inc)

    def CP(self, e, out, in_, reads, writes):
        nc = self.nc
        if e == "act":
            return self.S.op("act", lambda: nc.scalar.activation(out=out, in_=in_, func=AF.Copy), reads, writes)
        en = self.eng(e)
        return self.S.op(e, lambda: en.tensor_copy(out=out, in_=in_), reads, writes)

    def MS(self, e, ap, val, writes):
        en = self.eng(e)
        return self.S.op(e, lambda: en.memset(ap, val), (), writes)

    def dump(self, name, ap_sbuf, shape, reads):
        if name not in self.dbg:
            return
        o = self.dout("dbg_" + name, shape)
        self.S.dma("sp", o, ap_sbuf, reads=reads)

    def setup(self, es):
        nc = self.nc
        self.S = Sched(nc, es)
        S = self.S
        I = self.ins
        self.ps = [nc.alloc_psum_tensor(f"ps{i}", [128, 1024], F32).ap() for i in range(4)]
        sb = lambda n, sh, dt=F32: self.sb(es, n, sh, dt)
        self.ident = sb("ident", [128, 128])
        S.dma("sp", self.ident, I["c_ident"], writes=["ident"])
        self.onesd = sb("onesd", [128, 128])
        self.MS("pool", self.onesd, 1.0 / D, ["onesd"])
        self.bd1 = sb("bd1", [128, 128])
        self.MS("pool", self.bd1, 0.0, ["bd1"])
        self.MS("pool", self.bd1[0:64, 0:64], 1.0, ["bd1"])
        self.MS("pool", self.bd1[64:128, 64:128], 1.0, ["bd1"])
        self.eps6 = sb("eps6", [128, 1])
        self.MS("pool", self.eps6, 1e-6, ["eps6"])
        self.mod = sb("mod", [128, 48, 2])
        self.gs1 = sb("gs1", [128, 8, 2]); self.gs2 = sb("gs2", [128, 8, 2])
        self.gmix = sb("gmix", [128, 8]); self.gffn = sb("gffn", [128, 8]); self.gfin = sb("gfin", [128, 8])
        S.dma("sp", self.gfin, I["final_g"], writes=["gfin"])
        if self.fused:
            self.ohs = sb("ohs", [128, 2]); self.ohp = sb("ohp", [128, 2])
            S.dma("sp", self.ohs, I["oh_self"], writes=["ohs"])
            S.dma("sp", self.ohp, I["oh_part"], writes=["ohp"])

    def adaln(self):
        nc, S, I = self.nc, self.S, self.ins
        with contextlib.ExitStack() as st:
            cond = self.sb(st, "cond", [128, 8, 2])
            adab = self.sb(st, "adab", [128, 48])
            wbuf = [self.sb(st, f"adaw{i}", [128, 8, D]) for i in range(2)]
            S.dma("sp", self.gmix, I["g_mix"], writes=["gmix"])
            S.dma("sp", self.gffn, I["g_ffn"], writes=["gffn"])
            S.dma("sp", cond, I["cvec"], writes=["cond"])
            S.dma("sp", adab, I["ada_b"], writes=["adab"])
            self.A("act", cond, cond, AF.Silu, ["cond"], ["cond"])
            pst = self.ps[0][:, 0:96]
            for k in range(6):
                wb = wbuf[k % 2]
                wk = ("adaw", k % 2)
                S.dma("sp", wb, I["ada_w"][:, k * D:(k + 1) * D].rearrange("(c p) n -> p c n", p=128), writes=[wk])
                for fc in range(8):
                    col = (k * 8 + fc) * 2
                    for dc in range(8):
                        self.MM(pst[:, col:col + 2], wb[:, dc, fc * 128:(fc + 1) * 128], cond[:, dc, :],
                                dc == 0, dc == 7, [wk, "cond"], [("ps", 0, 0)])
            mod2 = self.mod.rearrange("p k j -> p (k j)")
            b3 = bass.AP(adab.tensor, adab.offset, [list(adab.ap[0]), [1, 48], [0, 2]])
            self.TT("dve", self.mod, pst.rearrange("p (k j) -> p k j", j=2), b3, ALU.add, [("ps", 0, 0), "adab"], ["mod"])
            for (gs, g, k, nm) in ((self.gs1, self.gmix, 1, "gs1"), (self.gs2, self.gffn, 4, "gs2")):
                g3 = bass.AP(g.tensor, g.offset, [list(g.ap[0]), [1, 8], [0, 2]])
                self.TS("dve", gs, self.mod[:, k * 8:(k + 1) * 8, :], 1.0, None, ALU.add, None, ["mod"], [nm])
                self.TT("dve", gs, gs, g3, ALU.mult, [nm, "gmix", "gffn"], [nm])
            self.dump("mod", self.mod, [128, 48, 2], ["mod"])
            S.barrier()

    def norm_tile(self, xt, xkey, T, gs, shift_k, j, out_fn, tag):
        nc, S = self.nc, self.S
        ps = self.ps[1][:, 0:T]
        pk = ("ps", 1, 0)
        for c in range(8):
            sq = self.nsq[c % 2][:, 0:T]
            self.A("act", sq, xt[:, c, :], AF.Square, [xkey], [("nsq", c % 2)])
            self.MM(ps, self.onesd, sq, c == 0, c == 7, ["onesd", ("nsq", c % 2)], [pk], inc=True)
        rstd = self.nrstd[:, 0:T]
        self.A("act", rstd, ps, AF.Sqrt, [pk, "eps6"], ["nrstd"], bias=self.eps6[:, 0:1], scale=1.0)
        S.op("dve", lambda: nc.vector.reciprocal(out=rstd, in_=rstd), ["nrstd"], ["nrstd"])
        for c in range(8):
            tmp = self.ntmp[c % 2][:, 0:T]
            self.TT("pool" if c % 2 else "dve", tmp, xt[:, c, :], rstd, ALU.mult, [xkey, "nrstd"], [("ntmp", c % 2)])
            out_fn(c, tmp, ("ntmp", c % 2))

    def norm1(self, hT):
        nc, S, I = self.nc, self.S, self.ins
        with contextlib.ExitStack() as st:
            xb = [self.sb(st, f"n1x{i}", [128, 8, 512]) for i in range(2)]
            self.nsq = [self.sb(st, f"nsq{i}", [128, 512]) for i in range(2)]
            self.ntmp = [self.sb(st, f"ntmp{i}", [128, 512]) for i in range(2)]
            self.nrstd = self.sb(st, "nrstd", [128, 512])
            for c in range(8):
                self.MS("pool", hT[:, c, 0:1], 0.0, ["hT"])
                self.MS("pool", hT[:, c, 257:258], 0.0, ["hT"])
            tiles = [(0, 256, CTX0, 1)] + [(CTX + i * 512, 512, LAT0 + i * 512, 0) for i in range(4)] + [(CTX + OWN, 256, HAL0, 0)]
            xv = self.x_src.rearrange("(c p) n -> p c n", p=128)
            for ti, (x0, T, h0, j) in enumerate(tiles):
                xt = xb[ti % 2]
                xk = ("n1x", ti % 2)
                S.dma("sp", xt[:, :, 0:T], xv[:, :, x0:x0 + T], writes=[xk])

                def out_fn(c, tmp, tk, h0=h0, T=T, j=j):
                    self.A("act", hT[:, c, h0:h0 + T], tmp, AF.Identity, [tk, "gs1", "mod"], ["hT"],
                           scale=self.gs1[:, c, j:j + 1], bias=self.mod[:, 0 * 8 + c, j:j + 1])
                self.norm_tile(xt[:, :, 0:T], xk, T, self.gs1, 0, j, out_fn, "n1")
            S.barrier()

    def load_w_bf16(self, dst, col0, ncols, key):
        src = self.ins["w_in"][:, col0:col0 + ncols].rearrange("(c p) n -> p c n", p=128)
        self.S.dma("pool", dst, src, writes=[key])

    def proj(self, ps, pkey, w, wkey, wcol0, M, hT, hcol0, T):
        for kc in range(8):
            self.MM(ps[0:M, 0:T], w[:, kc, wcol0:wcol0 + M], hT[:, kc, hcol0:hcol0 + T], kc == 0, kc == 7,
                    [wkey, "hT"], [pkey])

    def lru_setup(self, st):
        S, I = self.S, self.ins
        sb = lambda n, sh, dt=F32: self.sb(st, n, sh, dt)
        L = {}
        L["cw"] = sb("l_cw", [128, 2, 2, 4])
        S.dma("sp", L["cw"], I["lru_cw"], writes=["l_par"])
        for nm in ("cb", "br", "bi", "lam"):
            L[nm] = sb("l_" + nm, [128, 2, 2])
            S.dma("sp", L[nm], I["lru_" + nm], writes=["l_par"])
        L["wr"] = sb("l_wr", [128, 2, 2, 128]); L["wi"] = sb("l_wi", [128, 2, 2, 128])
        for nm in ("wr", "wi"):
            self.MS("pool", L[nm], 0.0, ["l_" + nm])
            for di in range(2):
                for blk in range(4):
                    cc, hh = blk // 2, blk % 2
                    S.dma("sp", L[nm][64 * hh:64 * hh + 64, di, cc, 64 * hh:64 * hh + 64], I["lru_" + nm][di, blk],
                          writes=["l_" + nm])
        L["c8"] = sb("l_c8", [128, 2, 2]); L["c16"] = sb("l_c16", [128, 2, 2])
        self.A("act", L["c8"], L["lam"], AF.Exp, ["l_par"], ["l_c8"], scale=-1.0)
        self.A("act", L["c8"], L["c8"], AF.Ln, ["l_c8"], ["l_c8"], bias=1.0, scale=1.0)
        self.TS("dve", L["c16"], L["c8"], -16.0, None, ALU.mult, None, ["l_c8"], ["l_c16"])
        self.TS("dve", L["c8"], L["c8"], -8.0, None, ALU.mult, None, ["l_c8"], ["l_c8"])
        L["hin"] = sb("l_hin", [128, 2])
        if not self.fused:
            S.dma("sp", L["hin"], I["lru_h_in"], writes=["l_hin"])
        L["w"] = sb("l_w", [128, 8, 512], BF16)
        self.load_w_bf16(L["w"], 0, 512, "l_w")
        self.L = L

    def lru_xa(self, st, hT):
        xa = self.sb(st, "l_xa", [128, 2, NXA])
        for cc in range(2):
            for (c0, n) in ((0, 3), (XC0 + 256, 3), (XL0 + 2048 + 3, NXA - (XL0 + 2048 + 3))):
                self.MS("pool", xa[:, cc, c0:c0 + n], 0.0, ["l_xa"])
        tiles = [(CTX0, 256, XC0)] + [(LAT0 + i * 512, 512, XL0 + i * 512) for i in range(4)] + [(HAL0, 3, XL0 + 2048)]
        i = 0
        for (h0, T, x0) in tiles:
            for cc in range(2):
                ps = self.ps[2 + (i % 2)]
                pk = ("ps", 2 + (i % 2), 0)
                self.proj(ps, pk, self.L["w"], "l_w", cc * 128, 128, hT, h0, T)
                self.CP("act" if i % 2 else "dve", xa[:, cc, x0:x0 + T], ps[:, 0:T], [pk], ["l_xa"])
                i += 1
        return xa

    def lru_pass(self, st, di, xa, hT, h1, mixT):
        nc, S, L = self.nc, self.S, self.L
        sb = lambda n, sh, dt=F32: self.sb(st, n, sh, dt)
        tg = f"lp{di}"
        nb = 2
        bufs = {nm: [sb(f"{tg}{nm}{i}", [128, 512]) for i in range(nb)] for nm in ("u", "r", "i", "a", "b", "h", "g")}
        sgn = 1 if di == 1 else -1
        seqs = [(XC0, 256, 0, CTX0), (XL0, 2048, 256, LAT0)]
        if di == 1:
            seqs = seqs[::-1]
            if self.skip_ctx_out:
                seqs = seqs[:1]
        it = 0
        for cc in range(2):
            carry = None
            for (x0, n, m0, h0) in seqs:
                T = min(512, n)
                tl = list(range(n // T))
                if di == 1:
                    tl = tl[::-1]
                    carry = L["hin"][:, cc:cc + 1] if x0 == XL0 else None
                elif x0 == XC0:
                    carry = None
                for ti in tl:
                    k = it % nb
                    kp = (it - 1) % nb
                    it += 1
                    c0 = x0 + ti * T
                    mc = m0 + ti * T
                    K_ = {nm: ("l" + nm, di, k) for nm in bufs}
                    ut, rt, itl, at, bt, ht, gt = (bufs[nm][k][:, 0:T] for nm in ("u", "r", "i", "a", "b", "h", "g"))
                    self.A("act", ut, xa[:, cc, c0:c0 + T], AF.Identity, ["l_xa", "l_par"], [K_["u"]],
                           scale=L["cw"][:, di, cc, 0:1], bias=L["cb"][:, di, cc:cc + 1])
                    for j in range(1, 4):
                        self.STT(ut, xa[:, cc, c0 + sgn * j:c0 + sgn * j + T], L["cw"][:, di, cc, j:j + 1], ut,
                                 ALU.mult, ALU.add, ["l_xa", "l_par", K_["u"]], [K_["u"]])
                    psr = self.ps[0]; psi = self.ps[1]
                    self.MM(psr[:, 0:T], L["wr"][:, di, cc, :], ut, True, True, ["l_wr", K_["u"]], [("ps", 0, 0)])
                    self.MM(psi[:, 0:T], L["wi"][:, di, cc, :], ut, True, True, ["l_wi", K_["u"]], [("ps", 1, 0)])
                    self.A("act", rt, psr[:, 0:T], AF.Sigmoid, [("ps", 0, 0), "l_par"], [K_["r"]], bias=L["br"][:, di, cc:cc + 1], scale=1.0)
                    self.A("act", itl, psi[:, 0:T], AF.Sigmoid, [("ps", 1, 0), "l_par"], [K_["i"]], bias=L["bi"][:, di, cc:cc + 1], scale=1.0)
                    self.A("act", at, rt, AF.Exp, [K_["r"], "l_c8"], [K_["a"]], scale=L["c8"][:, di, cc:cc + 1])
                    self.A("act", bt, rt, AF.Exp, [K_["r"], "l_c16"], [K_["b"]], scale=L["c16"][:, di, cc:cc + 1])
                    self.A("act", bt, bt, AF.Sqrt, [K_["b"]], [K_["b"]], scale=-1.0, bias=1.0)
                    self.TT("pool", itl, itl, ut, ALU.mult, [K_["i"], K_["u"]], [K_["i"]])
                    self.TT("pool", bt, bt, itl, ALU.mult, [K_["b"], K_["i"]], [K_["b"]])
                    init = 0.0 if carry is None else carry
                    if di == 0:
                        hdst = h1[:, cc, mc:mc + T]
                        S.op("dve", lambda hdst=hdst, at=at, bt=bt, init=init: nc.vector.tensor_tensor_scan(
                            out=hdst, data0=at, data1=bt, initial=init, op0=ALU.mult, op1=ALU.add),
                            [K_["a"], K_["b"], "l_h1"], ["l_h1"])
                        carry = h1[:, cc, mc + T - 1:mc + T]
                    else:
                        S.op("dve", lambda ht=ht, at=at, bt=bt, init=init: nc.vector.tensor_tensor_scan(
                            out=ht[:, ::-1], data0=at[:, ::-1], data1=bt[:, ::-1], initial=init, op0=ALU.mult, op1=ALU.add),
                            [K_["a"], K_["b"], "l_hin", ("lh", di, kp)], [K_["h"]])
                        carry = ht[:, 0:1]
                        psg = self.ps[2]
                        self.proj(psg, ("ps", 2, 0), L["w"], "l_w", 256 + cc * 128, 128, hT, h0 + ti * T, T)
                        self.A("act", gt, psg[:, 0:T], AF.Gelu, [("ps", 2, 0)], [K_["g"]])
                        self.TT("dve", bt, ht, h1[:, cc, mc:mc + T], ALU.add, [K_["h"], "l_h1", K_["b"]], [K_["b"]])
                        self.TT("pool", mixT[:, cc, mc:mc + T], gt, bt, ALU.mult, [K_["g"], K_["b"]], ["mixT"])

    def lru_stage(self, hT, mixT, phase):
        S = self.S
        with contextlib.ExitStack() as st:
            self.lru_setup(st)
            xa = self.lru_xa(st, hT)
            h1 = self.sb(st, "l_h1", [128, 2, NTOK])
            with contextlib.ExitStack() as st2:
                self.lru_pass(st2, 0, xa, hT, h1, mixT)
                S.barrier()
            if self.fused:
                hst = self.sb(st, "l_hst", [128, 2])
                for cc in range(2):
                    self.CP("dve", hst[:, cc:cc + 1], h1[:, cc, NTOK - 1:NTOK], ["l_h1"], ["l_hst"])
                self.xchg(f"lru{self.layer}", hst, self.L["hin"], 128, 2, "l_hst", "l_hin")
            if phase == "A":
                o = self.dout("lru_h_out", [128, 2])
                for cc in range(2):
                    S.dma("sp", o[:, cc:cc + 1], h1[:, cc, NTOK - 1:NTOK], reads=["l_h1"], allow_slow_non_contiguous=True)
            else:
                with contextlib.ExitStack() as st2:
                    self.lru_pass(st2, 1, xa, hT, h1, mixT)
                    S.barrier()
            S.barrier()

    def rwkv_setup(self, st):
        S, I, nc = self.S, self.ins, self.nc
        sb = lambda n, sh, dt=F32: self.sb(st, n, sh, dt)
        R = {}
        for nm in ("w0", "a0"):
            R[nm] = sb("r_" + nm, [128, 2, 2])
            S.dma("sp", R[nm], I["rwkv_" + nm], writes=["r_par"])
        for nm in ("kk", "ka", "rk", "lng", "lnb"):
            R[nm] = sb("r_" + nm, [128, 2])
            S.dma("sp", R[nm], I["rwkv_" + nm], writes=["r_par"])
        R["omka"] = sb("r_omka", [128, 2]); R["omka2"] = sb("r_omka2", [128, 2])
        self.TS("dve", R["omka"], R["ka"], -1.0, 1.0, ALU.mult, ALU.add, ["r_par"], ["r_omka"])
        self.TS("dve", R["omka2"], R["omka"], 2.0, None, ALU.mult, None, ["r_omka"], ["r_omka2"])
        R["lw"] = sb("r_lw", [128, 2, 256], BF16)
        for di in range(2):
            S.dma("pool", R["lw"][0:64, di, :], I["rwkv_w2"][di], writes=["r_lw"])
            S.dma("pool", R["lw"][64:128, di, :], I["rwkv_a2"][di], writes=["r_lw"])
        R["g2"] = sb("r_g2", [64, 256], BF16)
        S.dma("pool", R["g2"], I["rwkv_g2"], writes=["r_g2"])
        R["msk"] = sb("r_msk", [64, 3, 64])
        S.dma("sp", R["msk"], I["c_masks"], writes=["r_msk"])
        R["J"] = sb("r_J", [64, 64])
        self.CP("dve", R["J"], self.ident[0:64, 0:64][:, ::-1], ["ident"], ["r_J"])
        R["ones"] = sb("r_ones", [128, 64])
        self.MS("pool", R["ones"], 1.0, ["r_ones"])
        R["eps"] = sb("r_eps", [128, 1])
        self.MS("pool", R["eps"], 64e-5, ["r_eps"])
        R["Hin"] = sb("r_Hin", [64, 4, 64])
        if not self.fused:
            S.dma("sp", R["Hin"], I["rwkv_H_in"], writes=["r_Hin"])
        self.R = R

    def rwkv_stage0(self, st, hT, pb):
        S, I = self.S, self.ins
        sb = lambda n, sh, dt=F32: self.sb(st, n, sh, dt)
        wf = [sb(f"r0_wf{i}", [128, 8, 128]) for i in range(2)]
        mup = [sb(f"r0_mup{i}", [128, 128]) for i in range(2)]
        mun = [sb(f"r0_mun{i}", [128, 128]) for i in range(2)]
        c0 = [sb(f"r0_c0{i}", [128, 128]) for i in range(2)]
        Ws = [[sb(f"r0_W{i}{s}", [128, 8, 128], BF16) for s in range(3)] for i in range(2)]
        tiles = [(CTX0, 256, 0)] + [(LAT0 + i * 512, 512, 256 + i * 512) for i in range(4)]
        ip = 0
        for cc in range(8):
            k = cc % 2
            M = 128 if cc < 7 else 64
            col0 = 512 + cc * 128
            bc0 = cc * 128
            S.dma("sp", wf[k][:, :, 0:M], I["w_in"][:, col0:col0 + M].rearrange("(c p) n -> p c n", p=128), writes=[("r0wf", k)])
            for (t_, row, nm) in ((mup[k], 0, "r0mup"), (mun[k], 1, "r0mun")):
                src = I["rwkv_mu"][row:row + 1, bc0:bc0 + M]
                S.dma("sp", t_[:, 0:M], bass.AP(src.tensor, src.offset, [[0, 128], [1, M]]), writes=[(nm, k)])
            self.TT("dve", c0[k][:, 0:M], mup[k][:, 0:M], mun[k][:, 0:M], ALU.add, [("r0mup", k), ("r0mun", k)], [("r0c0", k)])
            self.TS("dve", c0[k][:, 0:M], c0[k][:, 0:M], -1.0, 1.0, ALU.mult, ALU.add, [("r0c0", k)], [("r0c0", k)])
            for s_, (sc, scn) in enumerate(((c0[k], "r0c0"), (mup[k], "r0mup"), (mun[k], "r0mun"))):
                sc3 = bass.AP(sc.tensor, sc.offset, [list(sc.ap[0]), [0, 8], [1, M]])
                self.TT("pool" if s_ == 1 else "dve", Ws[k][s_][:, :, 0:M], wf[k][:, :, 0:M], sc3, ALU.mult,
                        [("r0wf", k), (scn, k)], [("r0W", k, s_)])
            for (h0, T, p0) in tiles:
                ps = self.ps[2 + ip % 2]
                pk = ("ps", 2 + ip % 2, 0)
                ip += 1
                n = 0
                for s_, sh in enumerate((0, -1, 1)):
                    for kc in range(8):
                        self.MM(ps[0:M, 0:T], Ws[k][s_][:, kc, 0:M], hT[:, kc, h0 + sh:h0 + sh + T], n == 0, n == 23,
                                [("r0W", k, s_), "hT"], [pk])
                        n += 1
                dst = pb[:, cc, p0:p0 + T]
                if cc < 6:
                    self.CP("act" if ip % 2 else "dve", dst, ps[:, 0:T], [pk], ["r_pb"])
                elif cc == 6:
                    self.A("act", pb[0:64, cc, p0:p0 + T], ps[0:64, 0:T], AF.Tanh, [pk], ["r_pb"])
                    self.CP("dve", pb[64:128, cc, p0:p0 + T], ps[64:128, 0:T], [pk], ["r_pb"])
                else:
                    self.A("act", pb[0:64, cc, p0:p0 + T], ps[0:64, 0:T], AF.Sigmoid, [pk], ["r_pb"])

    def rwkv_pass(self, st, di, pb, y1, mixT, TB=128):
        nc, S, R = self.nc, self.S, self.R
        sb = lambda n, sh, dt=F32: self.sb(st, n, sh, dt)
        nC = TB // 64
        NB = nC * 4
        CL = 0.6065306597126334
        rev = di == 1
        tg = f"rp{di}_"
        names = ["sz", "a", "kk", "keff", "beta", "cs", "E1", "Em", "Ep", "Eh", "AT", "BT", "KT", "RT", "BhT", "KhT", "VT", "tmp", "tmp2"]
        if rev:
            names += ["a1", "ysum", "yc"]
        fm = {nm: [sb(tg + nm + str(hp), [128, TB]) for hp in range(2)] for nm in names}
        FK = lambda nm, hp: (tg + nm, hp)
        pc4 = sb(tg + "pc4", [64, 4, nC])
        opsT = {nm: [sb(tg + "o" + nm + str(h), [64, TB]) for h in range(4)] for nm in ("AT", "BT", "KT", "RT")}
        OK_ = lambda nm, h: (tg + "o" + nm, h)
        tok = sb(tg + "tok", [64, nC, 3, 2, 128])
        mats = {nm: sb(tg + nm, [64, NB, 64]) for nm in ("NT", "N", "Aak", "Gb", "Gk", "NT2", "N2", "Q", "Q2")}
        Xs = sb(tg + "Xs", [64, 256]); Us = sb(tg + "Us", [64, 256]); Ys = sb(tg + "Ys", [64, 256])
        H = sb(tg + "H", [64, 4, 64])
        y2 = sb(tg + "y2", [128, 2, TB])
        msk = R["msk"]
        ident64 = self.ident[0:64, 0:64]

        def v3(ap):
            a3 = ap.rearrange("p (c t) -> p c t", t=64)
            return a3[:, :, ::-1] if rev else a3

        if not rev:
            self.MS("pool", H, 0.0, [tg + "H"])
        else:
            self.CP("dve", H, R["Hin"], ["r_Hin"], [tg + "H"])
        tiles = [i * TB for i in range(NTOK // TB)]
        n_ctx_t = CTX // TB
        if rev:
            tiles = tiles[n_ctx_t:][::-1] + ([] if self.skip_ctx_out else tiles[:n_ctx_t][::-1])
        for tix, n0 in enumerate(tiles):
            if rev and tix == (NTOK - CTX) // TB:
                self.MS("pool", H, 0.0, [tg + "H"])
            th = pb[0:64, 6, n0:n0 + TB]; al = pb[64:128, 6, n0:n0 + TB]; sg = pb[0:64, 7, n0:n0 + TB]
            for hp in range(2):
                r_ = pb[:, 0 + hp, n0:n0 + TB]; k_ = pb[:, 2 + hp, n0:n0 + TB]; v_ = pb[:, 4 + hp, n0:n0 + TB]
                T_ = {nm: fm[nm][hp] for nm in names}
                pz = self.ps[0][:, 0:TB]; pa = self.ps[0][:, 512:512 + TB]; pss = self.ps[1][:, 0:TB]
                self.MM(pz, R["lw"][0:64, di, hp * 128:(hp + 1) * 128], th, True, True, ["r_lw", "r_pb"], [("ps", 0, 0)])
                self.A("act", T_["sz"], pz, AF.Sigmoid, [("ps", 0, 0), "r_par"], [FK("sz", hp)], bias=R["w0"][:, di, hp:hp + 1], scale=1.0)
                self.MM(pa, R["lw"][64:128, di, hp * 128:(hp + 1) * 128], al, True, True, ["r_lw", "r_pb"], [("ps", 0, 1)])
                self.A("act", T_["a"], pa, AF.Sigmoid, [("ps", 0, 1), "r_par"], [FK("a", hp)], bias=R["a0"][:, di, hp:hp + 1], scale=1.0)
                if rev:
                    self.MM(pa, R["lw"][64:128, 0, hp * 128:(hp + 1) * 128], al, True, True, ["r_lw", "r_pb"], [("ps", 0, 1)])
                    self.A("act", T_["a1"], pa, AF.Sigmoid, [("ps", 0, 1), "r_par"], [FK("a1", hp)], bias=R["a0"][:, 0, hp:hp + 1], scale=1.0)
                self.TS("dve", T_["kk"], k_, R["kk"][:, hp:hp + 1], None, ALU.mult, None, ["r_pb", "r_par"], [FK("kk", hp)])
                self.TT("dve", T_["tmp"], T_["kk"], T_["kk"], ALU.mult, [FK("kk", hp)], [FK("tmp", hp)])
                self.MM(pss, self.bd1, T_["tmp"], True, True, ["bd1", FK("tmp", hp)], [("ps", 1, 0)])
                self.A("act", T_["tmp2"], pss, AF.Sqrt, [("ps", 1, 0)], [FK("tmp2", hp)])
                self.TS("dve", T_["tmp2"], T_["tmp2"], 1e-12, None, ALU.max, None, [FK("tmp2", hp)], [FK("tmp2", hp)])
                S.op("dve", lambda o=T_["tmp2"]: nc.vector.reciprocal(out=o, in_=o), [FK("tmp2", hp)], [FK("tmp2", hp)])
                self.TT("dve", T_["kk"], T_["kk"], T_["tmp2"], ALU.mult, [FK("kk", hp), FK("tmp2", hp)], [FK("kk", hp)])
                self.TS("dve", T_["keff"], T_["a"], R["ka"][:, hp:hp + 1], R["omka"][:, hp:hp + 1], ALU.mult, ALU.add,
                        [FK("a", hp), "r_par", "r_omka"], [FK("keff", hp)])
                self.TT("dve", T_["keff"], T_["keff"], k_, ALU.mult, [FK("keff", hp), "r_pb"], [FK("keff", hp)])
                self.TT("dve", T_["beta"], T_["kk"], T_["a"], ALU.mult, [FK("kk", hp), FK("a", hp)], [FK("beta", hp)])
                for ch in range(nC):
                    src = v3(T_["sz"])[:, ch, :]; dst = v3(T_["cs"])[:, ch, :]
                    S.op("dve", lambda src=src, dst=dst: nc.vector.tensor_tensor_scan(
                        out=dst, data0=R["ones"][:, 0:64], data1=src, initial=0.0, op0=ALU.mult, op1=ALU.add),
                        [FK("sz", hp), "r_ones"], [FK("cs", hp)])
                cs3n = T_["cs"].rearrange("p (c t) -> p c t", t=64)
                csC = cs3n[:, :, 0:1] if rev else cs3n[:, :, 63:64]
                self.A("act", T_["E1"], T_["cs"], AF.Exp, [FK("cs", hp)], [FK("E1", hp)], scale=-CL)
                self.A("act", T_["Em"], T_["cs"], AF.Exp, [FK("cs", hp)], [FK("Em", hp)], scale=CL)
                self.TT("dve", T_["tmp"], T_["cs"], T_["sz"], ALU.subtract, [FK("cs", hp), FK("sz", hp)], [FK("tmp", hp)])
                self.A("act", T_["Ep"], T_["tmp"], AF.Exp, [FK("tmp", hp)], [FK("Ep", hp)], scale=-CL)
                csCb = bass.AP(csC.tensor, csC.offset, [list(csC.ap[0]), list(csC.ap[1]), [0, 64]])
                self.TT("dve", T_["tmp2"].rearrange("p (c t) -> p c t", t=64), csCb, cs3n, ALU.subtract, [FK("cs", hp)], [FK("tmp2", hp)])
                self.A("act", T_["Eh"], T_["tmp2"], AF.Exp, [FK("tmp2", hp)], [FK("Eh", hp)], scale=-CL)
                for hh in range(2):
                    h = 2 * hp + hh
                    rows = slice(64 * hh, 64 * hh + 64)
                    self.A("act", pc4[:, h, :], csC.rearrange("p c o -> p (c o)")[rows], AF.Exp, [FK("cs", hp)], [tg + "pc4"], scale=-CL)
                o3 = lambda nm: T_[nm].rearrange("p (c t) -> p c t", t=64)
                for hh in range(2):
                    h = 2 * hp + hh
                    rows = slice(64 * hh, 64 * hh + 64)
                    oh = lambda nm: opsT[nm][h].rearrange("p (c t) -> p c t", t=64)
                    S.op("dve", lambda oh=oh, rows=rows: nc.vector.scalar_tensor_tensor(out=oh("AT"), in0=v3(T_["kk"])[rows], scalar=-1.0, in1=v3(T_["Ep"])[rows],
                         op0=ALU.mult, op1=ALU.mult), [FK("kk", hp), FK("Ep", hp)], [OK_("AT", h)])
                    self.TT("dve", oh("BT"), v3(T_["beta"])[rows], v3(T_["Em"])[rows], ALU.mult, [FK("beta", hp), FK("Em", hp)], [OK_("BT", h)])
                    self.TT("dve", oh("KT"), v3(T_["keff"])[rows], v3(T_["Em"])[rows], ALU.mult, [FK("keff", hp), FK("Em", hp)], [OK_("KT", h)])
                    self.TT("dve", oh("RT"), v3(r_)[rows], v3(T_["E1"])[rows], ALU.mult, ["r_pb", FK("E1", hp)], [OK_("RT", h)])
                self.TT("dve", o3("BhT"), v3(T_["beta"]), v3(T_["Eh"]), ALU.mult, [FK("beta", hp), FK("Eh", hp)], [FK("BhT", hp)])
                self.TT("dve", o3("KhT"), v3(T_["keff"]), v3(T_["Eh"]), ALU.mult, [FK("keff", hp), FK("Eh", hp)], [FK("KhT", hp)])
                self.CP("act", o3("VT"), v3(v_), ["r_pb"], [FK("VT", hp)])
            if "rw_cut1" in self.dbg:
                return H
            for ch in range(nC):
                pt = self.ps[1]
                for j, nm in enumerate(("BhT", "KhT", "VT")):
                    for hp in range(2):
                        col = (j * 2 + hp) * 128
                        self.TR(pt[0:64, col:col + 128], fm[nm][hp][:, ch * 64:(ch + 1) * 64], self.ident,
                                ["ident", FK(nm, hp)], [("ps", 1, 0), ("ps", 1, 1)], inc=(j == 2 and hp == 1))
                self.CP("act", tok[:, ch].rearrange("p a b c -> p (a b c)"), pt[0:64, 0:768], [("ps", 1, 0), ("ps", 1, 1)], [tg + "tok"])
            if "rw_cut2" in self.dbg:
                return H
            def blk_ops(nm, hp, hh, ch):
                return opsT[nm][2 * hp + hh][:, ch * 64:(ch + 1) * 64]
            specs = (("NT", "AT", "BT", 0, 2), ("N", "BT", "AT", 1, 3), ("Aak", "KT", "AT", 1, 2), ("Gb", "BT", "RT", 2, 3), ("Gk", "KT", "RT", 2, 2))
            for (mn, ln, rn, mi, pi) in specs:
                pm = self.ps[pi][0:64, 0:NB * 64] if mn in ("NT", "Aak", "Gk") else self.ps[pi][0:64, 512:512 + NB * 64]
                pk = ("ps", pi, 0 if mn in ("NT", "Aak", "Gk") else 1)
                for ch in range(nC):
                    for h in range(4):
                        b_ = ch * 4 + h
                        hp, hh = h // 2, h % 2
                        self.MM(pm[:, b_ * 64:(b_ + 1) * 64], blk_ops(ln, hp, hh, ch), blk_ops(rn, hp, hh, ch), True, True,
                                [OK_(ln, h), OK_(rn, h)], [pk], inc=(b_ == NB - 1))
                mk = bass.AP(msk.tensor, msk[:, mi, :].offset, [list(msk.ap[0]), [0, NB], [1, 64]])
                self.TT("dve", mats[mn], pm.rearrange("p (b t) -> p b t", t=64), mk, ALU.mult, [pk, "r_msk"], [tg + mn])
            if "rw_cut3" in self.dbg:
                return H
            idb = bass.AP(ident64.tensor, ident64.offset, [list(ident64.ap[0]), [0, NB], [1, 64]])
            self.TT("dve", mats["Q"], mats["N"], idb, ALU.add, [tg + "N", "ident"], [tg + "Q"])
            cur = ("N", "NT", "Q"); nxt = ("N2", "NT2", "Q2")
            for kk_ in range(1, 6):
                pN = self.ps[2][0:64, 0:NB * 64]; pNT = self.ps[2][0:64, 512:512 + NB * 64]; pQ = self.ps[3][0:64, 0:NB * 64]
                last = kk_ == 5
                for b_ in range(NB):
                    if not last:
                        self.MM(pN[:, b_ * 64:(b_ + 1) * 64], mats[cur[1]][:, b_, :], mats[cur[0]][:, b_, :], True, True,
                                [tg + cur[0], tg + cur[1]], [("ps", 2, 0)], inc=(b_ == NB - 1))
                for b_ in range(NB):
                    self.MM(pNT[:, b_ * 64:(b_ + 1) * 64], mats[cur[0]][:, b_, :], mats[cur[1]][:, b_, :], True, True,
                            [tg + cur[0], tg + cur[1]], [("ps", 2, 1)], inc=(b_ == NB - 1))
                if not last:
                    self.CP("act", mats[nxt[0]], pN.rearrange("p (b t) -> p b t", t=64), [("ps", 2, 0)], [tg + nxt[0]])
                self.CP("dve", mats[nxt[1]], pNT.rearrange("p (b t) -> p b t", t=64), [("ps", 2, 1)], [tg + nxt[1]])
                for b_ in range(NB):
                    self.MM(pQ[:, b_ * 64:(b_ + 1) * 64], mats[nxt[1]][:, b_, :], mats[cur[2]][:, b_, :], True, True,
                            [tg + nxt[1], tg + cur[2]], [("ps", 3, 0)], inc=(b_ == NB - 1))
                self.TT("dve", mats[nxt[2]], mats[cur[2]], pQ.rearrange("p (b t) -> p b t", t=64), ALU.add, [tg + cur[2], ("ps", 3, 0)], [tg + nxt[2]])
                cur, nxt = nxt, cur
            Qn = cur[2]
            if "rw_cut4" in self.dbg:
                return H
            chs = list(range(nC))[::-1] if rev else list(range(nC))
            for ch in chs:
                pX = self.ps[3][0:64, 512:768]; pU = self.ps[3][0:64, 768:1024]
                pY = self.ps[0][0:64, 0:256]; pH = self.ps[0][0:64, 512:768]
                def Vtok(h):
                    return tok[:, ch, 2, h // 2, 64 * (h % 2):64 * (h % 2) + 64]
                for h in range(4):
                    hp, hh = h // 2, h % 2
                    b_ = ch * 4 + h
                    Hh = H[:, h, :]
                    self.MM(pX[:, h * 64:(h + 1) * 64], blk_ops("AT", hp, hh, ch), Hh, True, False, [OK_("AT", h), tg + "H"], [("ps", 3, 1)], inc=False)
                    self.MM(pX[:, h * 64:(h + 1) * 64], mats["Aak"][:, b_, :], Vtok(h), False, True, [tg + "Aak", tg + "tok"], [("ps", 3, 1)], inc=(h == 3))
                self.CP("act", Xs, pX, [("ps", 3, 1)], [tg + "Xs"])
                for h in range(4):
                    b_ = ch * 4 + h
                    self.MM(pU[:, h * 64:(h + 1) * 64], mats[Qn][:, b_, :], Xs[:, h * 64:(h + 1) * 64], True, True, [tg + Qn, tg + "Xs"], [("ps", 3, 1)], inc=(h == 3))
                self.CP("dve", Us, pU, [("ps", 3, 1)], [tg + "Us"])
                for h in range(4):
                    hp, hh = h // 2, h % 2
                    b_ = ch * 4 + h
                    Hh = H[:, h, :]
                    ysl = pY[:, h * 64:(h + 1) * 64]
                    self.MM(ysl, blk_ops("RT", hp, hh, ch), Hh, True, False, [OK_("RT", h), tg + "H"], [("ps", 0, 0)], inc=False)
                    self.MM(ysl, mats["Gb"][:, b_, :], Us[:, h * 64:(h + 1) * 64], False, False, [tg + "Gb", tg + "Us"], [("ps", 0, 0)], inc=False)
                    self.MM(ysl, mats["Gk"][:, b_, :], Vtok(h), False, True, [tg + "Gk", tg + "tok"], [("ps", 0, 0)], inc=(h == 3))
                for h in range(4):
                    hp, hh = h // 2, h % 2
                    hsl = pH[:, h * 64:(h + 1) * 64]
                    self.MM(hsl, tok[:, ch, 0, hp, 64 * hh:64 * hh + 64], Us[:, h * 64:(h + 1) * 64], True, False, [tg + "tok", tg + "Us"], [("ps", 0, 1)], inc=False)
                    self.MM(hsl, tok[:, ch, 1, hp, 64 * hh:64 * hh + 64], Vtok(h), False, True, [tg + "tok"], [("ps", 0, 1)], inc=(h == 3))
                self.CP("act", Ys, pY, [("ps", 0, 0)], [tg + "Ys"])
                for h in range(4):
                    self.STT(H[:, h, :], H[:, h, :], pc4[:, h, ch:ch + 1], pH[:, h * 64:(h + 1) * 64], ALU.mult, ALU.add,
                             [tg + "H", tg + "pc4", ("ps", 0, 1)], [tg + "H"])
                pT = self.ps[1][:, 512:640]
                for hp in range(2):
                    if rev:
                        self.MM(pT[:, hp * 64:(hp + 1) * 64], Ys[:, hp * 128:(hp + 1) * 128], R["J"], True, True, [tg + "Ys", "r_J"], [("ps", 1, 1)], inc=(hp == 1))
                    else:
                        self.TR(pT[:, hp * 64:(hp + 1) * 64], Ys[:, hp * 128:(hp + 1) * 128], ident64, [tg + "Ys", "ident"], [("ps", 1, 1)], inc=(hp == 1))
                ydst = y2[:, :, ch * 64:(ch + 1) * 64] if rev else y1[:, :, n0 + ch * 64:n0 + (ch + 1) * 64]
                self.CP("dve", ydst, pT.rearrange("p (a t) -> p a t", t=64), [("ps", 1, 1)], [tg + "y2" if rev else "r_y1"])
            if "rw_cut5" in self.dbg:
                return H
            if not rev:
                continue
            for hp in range(2):
                T_ = {nm: fm[nm][hp] for nm in names}
                r_ = pb[:, 0 + hp, n0:n0 + TB]; k_ = pb[:, 2 + hp, n0:n0 + TB]; v_ = pb[:, 4 + hp, n0:n0 + TB]
                p1 = self.ps[2][:, 0:TB]; p2 = self.ps[2][:, 512:512 + TB]; p3 = self.ps[3][:, 0:TB]; p4 = self.ps[1][:, 0:TB]
                self.TT("dve", T_["ysum"], y2[:, hp, :], y1[:, hp, n0:n0 + TB], ALU.add, [tg + "y2", "r_y1"], [FK("ysum", hp)])
                self.MM(p1, self.bd1, T_["ysum"], True, True, ["bd1", FK("ysum", hp)], [("ps", 2, 0)])
                self.STT(T_["yc"], p1, -1.0 / 64, T_["ysum"], ALU.mult, ALU.add, [("ps", 2, 0), FK("ysum", hp)], [FK("yc", hp)])
                self.TT("dve", T_["tmp"], T_["yc"], T_["yc"], ALU.mult, [FK("yc", hp)], [FK("tmp", hp)])
                self.MM(p2, self.bd1, T_["tmp"], True, True, ["bd1", FK("tmp", hp)], [("ps", 2, 1)])
                self.A("act", T_["tmp2"], p2, AF.Sqrt, [("ps", 2, 1), "r_eps"], [FK("tmp2", hp)], scale=1.0 / 64, bias=R["eps"][:, 0:1])
                S.op("dve", lambda o=T_["tmp2"]: nc.vector.reciprocal(out=o, in_=o), [FK("tmp2", hp)], [FK("tmp2", hp)])
                self.TT("dve", T_["yc"], T_["yc"], T_["tmp2"], ALU.mult, [FK("yc", hp), FK("tmp2", hp)], [FK("yc", hp)])
                self.TS("dve", T_["yc"], T_["yc"], R["lng"][:, hp:hp + 1], R["lnb"][:, hp:hp + 1], ALU.mult, ALU.add, [FK("yc", hp), "r_par"], [FK("yc", hp)])
                self.TT("dve", T_["tmp"], T_["a"], T_["a1"], ALU.add, [FK("a", hp), FK("a1", hp)], [FK("tmp", hp)])
                self.TS("dve", T_["tmp"], T_["tmp"], R["ka"][:, hp:hp + 1], R["omka2"][:, hp:hp + 1], ALU.mult, ALU.add, [FK("tmp", hp), "r_par", "r_omka2"], [FK("tmp", hp)])
                self.TT("dve", T_["tmp2"], r_, k_, ALU.mult, ["r_pb"], [FK("tmp2", hp)])
                self.STT(T_["tmp"], T_["tmp2"], R["rk"][:, hp:hp + 1], T_["tmp"], ALU.mult, ALU.mult, [FK("tmp2", hp), FK("tmp", hp), "r_par"], [FK("tmp", hp)])
                self.MM(p3, self.bd1, T_["tmp"], True, True, ["bd1", FK("tmp", hp)], [("ps", 3, 0)])
                self.TT("dve", T_["tmp2"], p3, v_, ALU.mult, [("ps", 3, 0), "r_pb"], [FK("tmp2", hp)])
                self.TT("dve", T_["yc"], T_["yc"], T_["tmp2"], ALU.add, [FK("yc", hp), FK("tmp2", hp)], [FK("yc", hp)])
                self.MM(p4, R["g2"][:, hp * 128:(hp + 1) * 128], sg, True, True, ["r_g2", "r_pb"], [("ps", 1, 0)])
                self.TT("dve", mixT[:, 2 + hp, n0:n0 + TB], T_["yc"], p4, ALU.mult, [FK("yc", hp), ("ps", 1, 0)], ["mixT"])
        return H

    def rwkv_stage(self, hT, mixT, phase):
        S = self.S
        with contextlib.ExitStack() as st:
            self.rwkv_setup(st)
            pb = self.sb(st, "r_pb", [128, 8, NTOK], BF16)
            with contextlib.ExitStack() as st2:
                self.rwkv_stage0(st2, hT, pb)
                S.barrier()
            y1 = self.sb(st, "r_y1", [128, 2, NTOK])
            if "rw_cut0" in self.dbg:
                S.barrier()
                return
            with contextlib.ExitStack() as st2:
                H = self.rwkv_pass(st2, 0, pb, y1, mixT)
                if phase == "A":
                    o = self.dout("rwkv_H_out", [64, 4, 64])
                    S.dma("sp", o, H, reads=["rp0_H"])
                S.barrier()
                if self.fused:
                    self.xchg(f"rwkv{self.layer}", H.rearrange("p a b -> p (a b)"), self.R["Hin"].rearrange("p a b -> p (a b)"), 64, 256, "rp0_H", "r_Hin")
            if phase != "A":
                with contextlib.ExitStack() as st2:
                    self.rwkv_pass(st2, 1, pb, y1, mixT)
                    S.barrier()
            S.barrier()

    def na_stage(self, hT, mixT):
        nc, S, I = self.nc, self.S, self.ins
        with contextlib.ExitStack() as st:
            sb = lambda n, sh, dt=F32: self.sb(st, n, sh, dt)
            wq = sb("n_wq", [128, 8, 128], BF16); wk = sb("n_wk", [128, 8, 128], BF16); wv = sb("n_wv", [128, 8, 128], BF16)
            qT = sb("n_qT", [128, NTOK], BF16)
            kT = sb("n_kT", [128, NEXT], BF16)
            V = sb("n_V", [128, 20, 2, 65], BF16)
            E = sb("n_E", [128, 3, 2, 640], BF16)
            stg = [sb(f"n_stg{i}", [128, 640]) for i in range(2)]
            Pb = [sb(f"n_P{i}", [128, 896], BF16) for i in range(2)]
            ysb = [sb(f"n_y{i}", [128, 128]) for i in range(2)]
            rc = [sb(f"n_rc{i}", [128, 2]) for i in range(2)]
            self.MS("pool", V[:, :, :, 64:65], 1.0, ["n_V1"])
            ip = 0
            for hp in range(4):
                self.load_w_bf16(wq, 1472 + 128 * hp, 128, "n_wq")
                self.load_w_bf16(wk, 1984 + 128 * hp, 128, "n_wk")
                self.load_w_bf16(wv, 2496 + 128 * hp, 128, "n_wv")
                for (h0, T, q0) in [(CTX0, 256, 0)] + [(LAT0 + i * 512, 512, 256 + i * 512) for i in range(4)]:
                    ps = self.ps[3]; pk = ("ps", 3, 0)
                    self.proj(ps, pk, wq, "n_wq", 0, 128, hT, h0, T)
                    self.A("act", qT[:, q0:q0 + T], ps[:, 0:T], AF.Copy, [pk], ["n_qT"], scale=0.125)
                for (h0, T, k0) in [(LAT0 + i * 512, 512, i * 512) for i in range(4)] + [(HAL0, 256, 2048), (CTX0, 256, 2304)]:
                    ps = self.ps[3][:, 512:1024]; pk = ("ps", 3, 1)
                    self.proj(ps, pk, wk, "n_wk", 0, 128, hT, h0, T)
                    self.CP("dve", kT[:, k0:k0 + T], ps[:, 0:T], [pk], ["n_kT"])
                for tt in range(20):
                    h0 = LAT0 + 128 * tt if tt < 16 else (HAL0 + 128 * (tt - 16) if tt < 18 else CTX0 + 128 * (tt - 18))
                    ps = self.ps[2][:, 512:640]; pk = ("ps", 2, 1)
                    for kc in range(8):
                        self.MM(ps, hT[:, kc, h0:h0 + 128], wv[:, kc, :], kc == 0, kc == 7, ["hT", "n_wv"], [pk])
                    self.CP("act" if tt % 2 else "dve", V[:, tt, :, 0:64], ps.rearrange("p (a d) -> p a d", d=64), [pk], ["n_V"])
                for cls in range(3):
                    for hh in range(2):
                        k = ip % 2; ip += 1
                        S.dma("sp", stg[k].rearrange("p (j q) -> p j q", q=128),
                              I["na_tab"][cls, 2 * hp + hh].rearrange("(j p) q -> p j q", p=128), writes=[("n_stg", k)])
                        self.A("act", E[:, cls, hh, :], stg[k], AF.Exp, [("n_stg", k)], ["n_E"])
                units = ([] if self.skip_ctx_out else [("c", 0), ("c", 1)]) + [("l", rp) for rp in range(16)]
                for ui, (kind, rp) in enumerate(units):
                    yk = ui % 2
                    psO = self.ps[2][:, 0:130]; pko = ("ps", 2, 0)
                    if kind == "l":
                        cls = min(rp, 2); base = max(rp - 2, 0)
                        kcols = [(base + j) * 128 for j in range(5)] + [2304, 2432]
                        vt = [base + j for j in range(5)] + [18, 19]
                        qc = 256 + rp * 128
                    else:
                        kcols = [2304, 2432]; vt = [18, 19]; qc = rp * 128
                    nk = len(kcols)
                    for hh in range(2):
                        psS = self.ps[hh]
                        pks = [("ps", hh, 0), ("ps", hh, 1)]
                        hs = slice(64 * hh, 64 * hh + 64)
                        for j, kc0 in enumerate(kcols):
                            self.MM(psS[:, j * 128:(j + 1) * 128], kT[hs, kc0:kc0 + 128], qT[hs, qc:qc + 128], True, True,
                                    ["n_kT", "n_qT"], [pks[j // 4]], inc=(j == nk - 1 or j == 3))
                    for hh in range(2):
                        psS = self.ps[hh]
                        pks = [("ps", hh, 0), ("ps", hh, 1)]
                        P = Pb[hh]
                        self.A("act", P[:, 0:nk * 128], psS[:, 0:nk * 128], AF.Exp, pks[0:(2 if nk > 4 else 1)], [("n_P", hh)])
                        if kind == "l":
                            self.TT("dve", P[:, 0:640], P[:, 0:640], E[:, cls, hh, :], ALU.mult, [("n_P", hh), "n_E"], [("n_P", hh)])
                    for hh in range(2):
                        P = Pb[hh]
                        for j in range(nk):
                            self.MM(psO[:, hh * 65:(hh + 1) * 65], P[:, j * 128:(j + 1) * 128], V[:, vt[j], hh, :], j == 0, j == nk - 1,
                                    [("n_P", hh), "n_V", "n_V1"], [pko], inc=(j == nk - 1 and hh == 1))
                    o3 = psO.rearrange("p (a d) -> p a d", d=65)
                    S.op("dve", lambda yk=yk, o3=o3: nc.vector.reciprocal(out=rc[yk].rearrange("p (a o) -> p a o", o=1), in_=o3[:, :, 64:65]), [pko], [("n_rc", yk)])
                    for hh in range(2):
                        self.TS("dve", ysb[yk][:, hh * 64:(hh + 1) * 64], psO[:, hh * 65:hh * 65 + 64], rc[yk][:, hh:hh + 1], None, ALU.mult, None,
                                [pko, ("n_rc", yk)], [("n_y", yk)])
                    pT = self.ps[3][:, 0:128] if yk == 0 else self.ps[3][:, 512:640]
                    pkt = ("ps", 3, yk)
                    self.TR(pT, ysb[yk], self.ident, [("n_y", yk), "ident"], [pkt])
                    mc = 256 + rp * 128 if kind == "l" else rp * 128
                    self.CP("act", mixT[:, 4 + hp, mc:mc + 128], pT, [pkt], ["mixT"])
            S.barrier()

    TOK_TILES = [(0, 256, 1)] + [(256 + i * 512, 512, 0) for i in range(4)]

    def wout_stage(self, xT, mixT):
        S, I = self.S, self.ins
        with contextlib.ExitStack() as st:
            wo = self.sb(st, "wo", [128, 8, D], BF16)
            S.dma("pool", wo, I["w_out"].rearrange("(c p) n -> p c n", p=128), writes=["wo"])
            xv = self.x_src.rearrange("(c p) n -> p c n", p=128)
            for c in range(8):
                S.dma("sp", xT[:, c, :], xv[:, c, 0:NTOK], writes=["xT"])
            i = 0
            for (c0, T, j) in self.tok_tiles:
                for dc in range(8):
                    ps = self.ps[i % 4][:, 0:T]; pk = ("ps", i % 4, 0)
                    i += 1
                    for fc in range(8):
                        self.MM(ps, wo[:, fc, dc * 128:(dc + 1) * 128], mixT[:, fc, c0:c0 + T], fc == 0, fc == 7, ["wo", "mixT"], [pk])
                    self.STT(xT[:, dc, c0:c0 + T], ps, self.mod[:, 16 + dc, j:j + 1], xT[:, dc, c0:c0 + T], ALU.mult, ALU.add,
                             [pk, "mod", "xT"], ["xT"])
            S.barrier()

    def norm2_router(self, st, xT, h2T, gT):
        nc, S, I = self.nc, self.S, self.ins
        with contextlib.ExitStack() as st2:
            sb = lambda n, sh, dt=F32: self.sb(st2, n, sh, dt)
            self.nsq = [sb(f"m_nsq{i}", [128, 512]) for i in range(2)]
            self.ntmp = [sb(f"m_ntmp{i}", [128, 512]) for i in range(2)]
            self.nrstd = sb("m_nrstd", [128, 512])
            h2f = sb("m_h2f", [128, 8, 512])
            rw = sb("m_rw", [128, 8, 32])
            S.dma("sp", rw, I["router_w"].rearrange("(c p) e -> p c e", p=128), writes=["m_rw"])
            rb = sb("m_rb", [128, 32])
            src = I["router_b"]
            S.dma("sp", rb, bass.AP(src.tensor, src.offset, [[0, 128], [1, 32]]), writes=["m_rb"])
            lg = sb("m_lg", [128, 32]); t8 = sb("m_t8", [128, 8]); mk = sb("m_mk", [128, 32]); ex = sb("m_ex", [128, 32])
            nm = sb("m_nm", [128, 1]); ss = sb("m_ss", [128, 1])
            for (c0, T, j) in self.tok_tiles:
                def out_fn(c, tmp, tk, c0=c0, T=T, j=j):
                    self.A("act", h2f[:, c, 0:T], tmp, AF.Identity, [tk, "gs2", "mod"], ["m_h2f"],
                           scale=self.gs2[:, c, j:j + 1], bias=self.mod[:, 24 + c, j:j + 1])
                    self.CP("pool" if c % 2 else "dve", h2T[:, c, c0:c0 + T], h2f[:, c, 0:T], ["m_h2f"], ["h2T"])
                self.norm_tile(xT[:, :, c0:c0 + T], "xT", T, self.gs2, 3, j, out_fn, "n2")
                for sub in range(T // 128):
                    pl = self.ps[2][:, 0:32]; pk = ("ps", 2, 0)
                    for c in range(8):
                        self.MM(pl, h2f[:, c, sub * 128:(sub + 1) * 128], rw[:, c, :], c == 0, c == 7, ["m_h2f", "m_rw"], [pk])
                    self.TT("dve", lg, pl, rb, ALU.add, [pk, "m_rb"], ["m_lg"])
                    S.op("dve", lambda: nc.vector.max(out=t8, in_=lg), ["m_lg"], ["m_t8"])
                    self.TS("dve", mk, lg, t8[:, 3:4], None, ALU.is_ge, None, ["m_lg", "m_t8"], ["m_mk"])
                    self.TS("dve", nm, t8[:, 0:1], -1.0, None, ALU.mult, None, ["m_t8"], ["m_nm"])
                    self.A("act", ex, lg, AF.Exp, ["m_lg", "m_nm"], ["m_ex"], bias=nm[:, 0:1], scale=1.0)
                    self.TT("dve", ex, ex, mk, ALU.mult, ["m_ex", "m_mk"], ["m_ex"])
                    S.op("dve", lambda: nc.vector.reduce_sum(out=ss, in_=ex, axis=AX.X), ["m_ex"], ["m_ss"])
                    S.op("dve", lambda: nc.vector.reciprocal(out=ss, in_=ss), ["m_ss"], ["m_ss"])
                    self.TS("dve", ex, ex, ss[:, 0:1], None, ALU.mult, None, ["m_ex", "m_ss"], ["m_ex"])
                    pt = self.ps[2][0:32, 512:640]; pkt = ("ps", 2, 1)
                    self.TR(pt, ex, self.ident, ["m_ex", "ident"], [pkt])
                    self.CP("act", gT[0:32, c0 + sub * 128:c0 + (sub + 1) * 128], pt, [pkt], ["gT"])
            S.barrier()

    def moe_stage(self, st, xT, h2T, gT):
        nc, S, I = self.nc, self.S, self.ins
        with contextlib.ExitStack() as st2:
            sb = lambda n, sh, dt=F32: self.sb(st2, n, sh, dt)
            ones32 = sb("e_ones", [32, 128])
            self.MS("pool", ones32, 1.0, ["e_ones"])
            gsel = sb("e_gsel", [32, 512])
            bgu = sb("e_bgu", [128, 32, 16])
            S.dma("sp", bgu, I["moe_b_gu"], writes=["e_bgu"])
            bdn = sb("e_bdn", [32, D])
            S.dma("sp", bdn, I["moe_b_dn"], writes=["e_bdn"])
            Gs = sb("e_Gs", [128, NTOK])
            wg = [sb(f"e_wg{i}", [128, 8, 512], BF16) for i in range(2)]
            wu = [sb(f"e_wu{i}", [128, 8, 512], BF16) for i in range(2)]
            wd = [sb(f"e_wd{i}", [128, 4, D], BF16) for i in range(2)]
            nb = 2
            gt = [sb(f"e_gt{i}", [128, 512]) for i in range(nb)]
            sg = [sb(f"e_sg{i}", [128, 512]) for i in range(nb)]
            ut = [sb(f"e_ut{i}", [128, 512]) for i in range(nb)]
            act = [sb(f"e_act{i}", [128, 4, 512], BF16) for i in range(2)]
            i = 0
            for (c0, T, j) in self.tok_tiles:
                for dc in range(8):
                    ps = self.ps[i % 4][:, 0:T]; pk = ("ps", i % 4, 0); i += 1
                    self.MM(ps, bdn[:, dc * 128:(dc + 1) * 128], gT[0:32, c0:c0 + T], True, True, ["e_bdn", "gT"], [pk])
                    self.STT(xT[:, dc, c0:c0 + T], ps, self.mod[:, 40 + dc, j:j + 1], xT[:, dc, c0:c0 + T], ALU.mult, ALU.add,
                             [pk, "mod", "xT"], ["xT"])
            wgu_v = I["moe_w_gu"]; wdn_v = I["moe_w_dn"]
            tiles = self.tok_tiles

            def load_piece(p):
                if p >= 64:
                    return
                e, half = p // 2, p % 2
                k = p % 2
                f0 = half * 512
                S.dma("pool", wg[k], wgu_v[e, :, f0:f0 + 512].rearrange("(c p) n -> p c n", p=128), writes=[("e_wg", k)])
                S.dma("pool", wu[k], wgu_v[e, :, D + f0:D + f0 + 512].rearrange("(c p) n -> p c n", p=128), writes=[("e_wu", k)])
                S.dma("pool", wd[k], wdn_v[e, f0:f0 + 512, :].rearrange("(c p) n -> p c n", p=128), writes=[("e_wd", k)])

            units = []
            for p in range(64):
                for ti, tl in enumerate(tiles):
                    units.append((p, ti, tl))
            cnt = {"it": 0}

            def emit_gu(n):
                p, ti, (c0, T, j) = units[n]
                e, half = p // 2, p % 2
                k = p % 2
                ak = n % 2
                if half == 0 and ti == 0:
                    for (c0g, Tg, jg) in tiles:
                        ps = self.ps[3][:, 512:512 + Tg]; pk = ("ps", 3, 1)
                        self.TS("dve", gsel[:, 0:Tg], gT[0:32, c0g:c0g + Tg], self.ident[0:32, e:e + 1], None, ALU.mult, None, ["gT", "ident"], ["e_gsel"])
                        self.MM(ps, ones32, gsel[:, 0:Tg], True, True, ["e_ones", "e_gsel"], [pk])
                        self.CP("act", Gs[:, c0g:c0g + Tg], ps, [pk], ["e_Gs"])
                for fci in range(4):
                    b_ = cnt["it"] % nb; cnt["it"] += 1
                    psg = self.ps[0][:, 0:T] if fci % 2 == 0 else self.ps[0][:, 512:512 + T]
                    psu = self.ps[1][:, 0:T] if fci % 2 == 0 else self.ps[1][:, 512:512 + T]
                    pkg = ("ps", 0, fci % 2); pku = ("ps", 1, fci % 2)
                    for kc in range(8):
                        self.MM(psg, wg[k][:, kc, fci * 128:(fci + 1) * 128], h2T[:, kc, c0:c0 + T], kc == 0, kc == 7, [("e_wg", k), "h2T"], [pkg])
                    for kc in range(8):
                        self.MM(psu, wu[k][:, kc, fci * 128:(fci + 1) * 128], h2T[:, kc, c0:c0 + T], kc == 0, kc == 7, [("e_wu", k), "h2T"], [pku])
                    fc16 = half * 4 + fci
                    g_ = gt[b_][:, 0:T]; s_ = sg[b_][:, 0:T]; u_ = ut[b_][:, 0:T]
                    self.TS("dve", g_, psg, bgu[:, e, fc16:fc16 + 1], 7.0, ALU.add, ALU.min, [pkg, "e_bgu"], [("e_gt", b_)])
                    self.A("act", s_, g_, AF.Sigmoid, [("e_gt", b_)], [("e_sg", b_)], scale=1.702)
                    self.TS("dve", u_, psu, bgu[:, e, 8 + fc16:8 + fc16 + 1], 7.0, ALU.add, ALU.min, [pku, "e_bgu"], [("e_ut", b_)])
                    self.TS("dve", u_, u_, -7.0, 1.0, ALU.max, ALU.add, [("e_ut", b_)], [("e_ut", b_)])
                    self.TT("dve", g_, g_, s_, ALU.mult, [("e_gt", b_), ("e_sg", b_)], [("e_gt", b_)])
                    self.TT("pool", u_, u_, Gs[:, c0:c0 + T], ALU.mult, [("e_ut", b_), "e_Gs"], [("e_ut", b_)])
                    self.TT("pool", act[ak][:, fci, 0:T], u_, g_, ALU.mult, [("e_ut", b_), ("e_gt", b_)], [("e_act", ak)])

            def emit_dn(n):
                p, ti, (c0, T, j) = units[n]
                k = p % 2
                ak = n % 2
                for dc in range(8):
                    pso = self.ps[2][:, 0:T] if dc % 2 == 0 else self.ps[2][:, 512:512 + T]
                    pko = ("ps", 2, dc % 2)
                    for fci in range(4):
                        self.MM(pso, wd[k][:, fci, dc * 128:(dc + 1) * 128], act[ak][:, fci, 0:T], fci == 0, fci == 3, [("e_wd", k), ("e_act", ak)], [pko])
                    self.STT(xT[:, dc, c0:c0 + T], pso, self.mod[:, 40 + dc, j:j + 1], xT[:, dc, c0:c0 + T], ALU.mult, ALU.add,
                             [pko, "mod", ("xT", dc)], [("xT", dc)])
                if ti == len(tiles) - 1:
                    load_piece(p + 2)

            load_piece(0); load_piece(1)
            emit_gu(0)
            for n in range(len(units)):
                if n + 1 < len(units):
                    emit_gu(n + 1)
                emit_dn(n)
            S.barrier()

    def final_stage(self, xT, last):
        nc, S, I = self.nc, self.S, self.ins
        with contextlib.ExitStack() as st2:
            sb = lambda n, sh, dt=F32: self.sb(st2, n, sh, dt)
            if not last:
                o = self.dout("xT_out", [D, NTOK])
                ov = o.rearrange("(c p) n -> p c n", p=128)
                for c in range(8):
                    S.dma("sp", ov[:, c, :], xT[:, c, :], reads=["xT"])
            else:
                self.nsq = [sb(f"f_nsq{i}", [128, 512]) for i in range(2)]
                self.ntmp = [sb(f"f_ntmp{i}", [128, 512]) for i in range(2)]
                self.nrstd = sb("f_nrstd", [128, 512])
                ob = [sb(f"f_ob{i}", [128, 8, 512]) for i in range(2)]
                o = self.dout("outT", [D, OWN])
                ov = o.rearrange("(c p) n -> p c n", p=128)
                for ti in range(4):
                    c0 = 256 + ti * 512
                    obt = ob[ti % 2]
                    def out_fn(c, tmp, tk, obt=obt, ti=ti):
                        self.TS("dve", obt[:, c, :], tmp, self.gfin[:, c:c + 1], None, ALU.mult, None, [tk, "gfin"], [("f_ob", ti % 2)])
                    self.norm_tile(xT[:, :, c0:c0 + 512], "xT", 512, None, None, 0, out_fn, "nf")
                    S.dma("sp", ov[:, :, ti * 512:(ti + 1) * 512], obt, reads=[("f_ob", ti % 2)])
            S.barrier()

    def handoff_stage(self, xT, xs1):
        S = self.S
        with contextlib.ExitStack() as st2:
            xv = xs1.rearrange("(c p) n -> p c n", p=128)
            for c in range(8):
                S.dma("sp", xv[:, c, 0:NTOK], xT[:, c, :], reads=["xT"], writes=["xs1"])
            blk = self.sb(st2, "ho_blk", [128, 8, 256]); got = self.sb(st2, "ho_got", [128, 8, 256]); rev = self.sb(st2, "ho_rev", [128, 8, 256])
            self.CP("dve", blk, xT[:, :, NTOK - 256:NTOK], ["xT"], ["ho_blk"])
            self.xchg("halo", blk.rearrange("p a b -> p (a b)"), got.rearrange("p a b -> p (a b)"), 128, 2048, "ho_blk", "ho_got")
            self.CP("dve", rev, got[:, :, ::-1], ["ho_got"], ["ho_rev"])
            for c in range(8):
                S.dma("sp", xv[:, c, NTOK:NEXT], rev[:, c, :], reads=["ho_rev"], writes=["xs1"])
            S.barrier()


def build_program(phase="B", last=False, dbg=(), stop_after=None):
    P = Prog(dbg)
    P.skip_ctx_out = "skip_ctx_out" in P.dbg
    if P.skip_ctx_out:
        P.tok_tiles = [(256 + i * 512, 512, 0) for i in range(4)]
    with contextlib.ExitStack() as es:
        P.setup(es)
        P.x_src = P.ins["xT"]
        P.adaln()
        mixT = P.sb(es, "mixT", [128, 8, NTOK], BF16)

        def dump_bf(stk, name, src, chunks, key):
            tmpf = P.sb(stk, "dbg_" + name, [128, src.shape[-1]])
            for c in chunks:
                o = P.dout(f"dbg_{name}{c}", [128, src.shape[-1]])
                P.CP("dve", tmpf, src[:, c, :], [key], ["dbgf"])
                P.S.dma("sp", o, tmpf, reads=["dbgf"])
            P.S.barrier()

        with contextlib.ExitStack() as sh:
            hT = P.sb(sh, "hT", [128, 8, NTP], BF16)
            P.norm1(hT)
            if stop_after == "norm1":
                dump_bf(sh, "hT", hT, range(8), "hT")
                return P
            if "skip_lru" not in P.dbg:
                P.lru_stage(hT, mixT, phase)
            if stop_after == "lru":
                if phase == "B":
                    dump_bf(sh, "mix", mixT, (0, 1), "mixT")
                P.S.barrier()
                return P
            if "skip_rwkv" not in P.dbg:
                P.rwkv_stage(hT, mixT, phase)
            if stop_after == "rwkv":
                if phase == "B":
                    dump_bf(sh, "mix", mixT, (2, 3), "mixT")
                P.S.barrier()
                return P
            if phase == "A":
                P.S.barrier()
                return P
            if "skip_na" not in P.dbg:
                P.na_stage(hT, mixT)
            if stop_after == "na":
                dump_bf(sh, "mix", mixT, range(8), "mixT")
                return P
            P.S.barrier()
        with contextlib.ExitStack() as sx:
            xT = P.sb(sx, "xres", [128, 8, NTOK])
            P.wout_stage(xT, mixT)
            if stop_after == "wout":
                o = P.dout("dbg_xmid", [D, NTOK])
                ov = o.rearrange("(c p) n -> p c n", p=128)
                for c in range(8):
                    P.S.dma("sp", ov[:, c, :], xT[:, c, :], reads=["xT"])
                P.S.barrier()
                return P
            h2T = mixT
            gT = P.sb(sx, "gT", [32, NTOK])
            P.norm2_router(sx, xT, h2T, gT)
            if stop_after == "norm2":
                o = P.dout("dbg_gT", [32, NTOK])
                P.S.dma("sp", o, gT, reads=["gT"])
                dump_bf(sx, "h2", h2T, range(8), "h2T")
                return P
            P.moe_stage(sx, xT, h2T, gT)
            P.final_stage(xT, last)
            P.S.barrier()
    return P


def build_fused():
    P = Prog(())
    P.fused = True
    with contextlib.ExitStack() as es:
        P.setup(es)
        xs1 = P.nc.dram_tensor("x_scratch1", [D, NEXT], F32).ap()
        P.layer = 0
        mixT = P.sb(es, "mixT", [128, 8, NTOK], BF16)
        for l in range(2):
            P.layer = l
            P.x_src = P.ins["xT"] if l == 0 else xs1
            if l == 1:
                P.tok_tiles = [(256 + i * 512, 512, 0) for i in range(4)]
                P.skip_ctx_out = True
            P.adaln()
            with contextlib.ExitStack() as sh:
                hT = P.sb(sh, "hT", [128, 8, NTP], BF16)
                P.norm1(hT)
                P.lru_stage(hT, mixT, "F")
                P.rwkv_stage(hT, mixT, "F")
                P.na_stage(hT, mixT)
                P.S.barrier()
            with contextlib.ExitStack() as sx:
                xT = P.sb(sx, "xres", [128, 8, NTOK])
                P.wout_stage(xT, mixT)
                gT = P.sb(sx, "gT", [32, NTOK])
                P.norm2_router(sx, xT, mixT, gT)
                P.moe_stage(sx, xT, mixT, gT)
                if l == 0:
                    P.handoff_stage(xT, xs1)
                else:
                    P.final_stage(xT, True)
                P.S.barrier()
    return P


_NA_IDX = {}


def _na_index(s):
    if s in _NA_IDX:
        return _NA_IDX[s]
    key = np.arange(640)
    q = np.arange(128)
    br, kc_l = key // 64, key % 64
    qr, qc_l = q // 64, q % 64
    ri = np.zeros((3, 640, 128), np.int64)
    ci = np.zeros((3, 640, 128), np.int64)
    ok = np.zeros((3, 640, 128), bool)
    for cls in range(3):
        qrow_l = 2 * cls + qr
        krow_l = br
        if s == 0:
            r, c, kr, kc = qrow_l[None, :], qc_l[None, :], krow_l[:, None], kc_l[:, None]
        else:
            r, c, kr, kc = 63 - qrow_l[None, :], 63 - qc_l[None, :], 63 - krow_l[:, None], 63 - kc_l[:, None]
        rs = np.clip(r - 4, 0, 56)
        cs = np.clip(c - 8, 0, 48)
        v = (kr >= rs) & (kr < rs + 8) & (kc >= cs) & (kc < cs + 16)
        ok[cls] = v
        ri[cls] = np.where(v, kr - r + 7, 0)
        ci[cls] = np.where(v, kc - c + 15, 0)
    _NA_IDX[s] = (ri, ci, ok)
    return _NA_IDX[s]


_CONSTS = {}


def _consts():
    if not _CONSTS:
        _CONSTS["c_ident"] = np.eye(128, dtype=np.float32)
        sel = np.zeros((32, 32, 128), np.float32)
        for e in range(32):
            sel[e, e, :] = 1.0
        _CONSTS["c_sel"] = sel.reshape(32, 32 * 128)
        i = np.arange(64)
        lo = (i[None, :] < i[:, None])
        up = (i[:, None] < i[None, :])
        upi = (i[:, None] <= i[None, :])
        _CONSTS["c_masks"] = np.stack([lo, up, upi], 1).astype(np.float32)
    return _CONSTS


def local_order(xl_b, xc_b, s):
    if s == 0:
        return xc_b, xl_b[0:2048], xl_b[2048:2304]
    return xc_b[::-1], xl_b[4095:2047:-1], xl_b[2047:1791:-1]


def prep_core(inp, l, core, xl, xc):
    b, s = core // 2, core % 2
    d1, d2 = s, 1 - s
    m = {}
    ctx_, own_, halo_ = local_order(xl[b], xc[b], s)
    m["xT"] = np.ascontiguousarray(np.concatenate([ctx_, own_, halo_], 0).T)
    def pc(v):
        v = np.asarray(v)
        n = v.shape[-1] // 128
        v = v.reshape(v.shape[:-1] + (n, 128))
        return np.moveaxis(v, -1, 0)
    m["cvec"] = np.stack([pc(inp["c"][b]), pc(inp["c_ctx"])], -1)
    m["ada_w"] = inp["ada_w"][l]; m["ada_b"] = pc(inp["ada_b"][l])
    m["g_mix"] = pc(inp["norm_mix_g"][l]); m["g_ffn"] = pc(inp["norm_ffn_g"][l])
    m["w_in"] = inp["w_in"][l]; m["w_out"] = inp["w_out"][l]
    dd = [d1, d2]
    m["lru_cw"] = np.moveaxis(pc(inp["lru_conv_w"][l][dd]), 2, 3)
    m["lru_cb"] = pc(inp["lru_conv_b"][l][dd])
    m["lru_br"] = pc(inp["lru_br"][l][dd]); m["lru_bi"] = pc(inp["lru_bi"][l][dd]); m["lru_lam"] = pc(inp["lru_lambda"][l][dd])
    m["lru_wr"] = inp["lru_wr"][l][dd]; m["lru_wi"] = inp["lru_wi"][l][dd]
    m["rwkv_mu"] = inp["rwkv_mu"][l][[0, 1]] if s == 0 else inp["rwkv_mu"][l][[1, 0]]
    m["rwkv_w0"] = pc(inp["rwkv_w0"][l][dd]); m["rwkv_a0"] = pc(inp["rwkv_a0"][l][dd])
    m["rwkv_w2"] = inp["rwkv_w2"][l][dd]; m["rwkv_a2"] = inp["rwkv_a2"][l][dd]
    m["rwkv_g2"] = inp["rwkv_g2"][l]
    m["rwkv_kk"] = pc(inp["rwkv_kk"][l]); m["rwkv_ka"] = pc(inp["rwkv_ka"][l]); m["rwkv_rk"] = pc(inp["rwkv_rk"][l].reshape(256))
    m["rwkv_lng"] = pc(inp["rwkv_lnx_g"][l]); m["rwkv_lnb"] = pc(inp["rwkv_lnx_b"][l])
    ri, ci, ok = _na_index(s)
    rpb = inp["na_rpb"][l]
    tab = rpb[:, ri, ci]
    tab = np.where(ok[None], tab, np.float32(-30000.0))
    m["na_tab"] = np.ascontiguousarray(tab.transpose(1, 0, 2, 3)).astype(np.float32)
    m["router_w"] = inp["router_w"][l]; m["router_b"] = inp["router_b"][l].reshape(1, 32)
    m["moe_w_gu"] = inp["moe_w_gu"][l]; m["moe_b_gu"] = pc(inp["moe_b_gu"][l])
    m["moe_w_dn"] = inp["moe_w_dn"][l]; m["moe_b_dn"] = inp["moe_b_dn"][l]
    m["final_g"] = pc(inp["final_g"])
    m.update(_consts())
    m["lru_h_in"] = np.zeros((128, 2), np.float32)
    m["rwkv_H_in"] = np.zeros((64, 4, 64), np.float32)
    return {k: np.ascontiguousarray(v, dtype=np.float32) for k, v in m.items()}


_PROGS = {}


def _prog(phase, last):
    k = (phase, last)
    if k not in _PROGS:
        _PROGS[k] = build_program(phase=phase, last=last)
    return _PROGS[k]


def _launch(P, maps):
    res = run_bass_kernel_spmd(P.nc, [{k: m[k] for k in P.ins} for m in maps], core_ids=list(range(8)))
    return res.results


def kernel_unfused(**inputs):
    inp = {k: np.asarray(v, dtype=np.float32) for k, v in inputs.items()}
    xl = inp["x"]
    xc = inp["ctx"]
    out = None
    for l in range(2):
        last = l == 1
        maps = [prep_core(inp, l, c, xl, xc) for c in range(8)]
        ra = _launch(_prog("A", False), maps)
        for c in range(8):
            maps[c]["lru_h_in"] = ra[c ^ 1]["lru_h_out"]
            maps[c]["rwkv_H_in"] = ra[c ^ 1]["rwkv_H_out"]
        rb = _launch(_prog("B", last), maps)
        if not last:
            xl_n = np.empty_like(xl)
            xc_n = np.empty_like(xc)
            for c in range(8):
                b, s = c // 2, c % 2
                xo = rb[c]["xT_out"].T
                if s == 0:
                    xl_n[b, 0:2048] = xo[256:]
                    xc_n[b] = xo[:256]
                else:
                    xl_n[b, 2048:4096] = xo[256:][::-1]
            xl, xc = xl_n, xc_n
        else:
            out = np.empty((4, 4096, D), np.float32)
            for c in range(8):
                b, s = c // 2, c % 2
                yo = rb[c]["outT"].T
                if s == 0:
                    out[b, 0:2048] = yo
                else:
                    out[b, 2048:4096] = yo[::-1]
    return out


_FUSED = {}


def kernel(**inputs):
    inp = {k: np.asarray(v, dtype=np.float32) for k, v in inputs.items()}
    if "P" not in _FUSED:
        _FUSED["P"] = build_fused()
    P = _FUSED["P"]
    maps = []
    for c in range(8):
        s = c % 2
        m0 = prep_core(inp, 0, c, inp["x"], inp["ctx"])
        m1 = prep_core(inp, 1, c, inp["x"], inp["ctx"])
        m = {}
        for k, v in m0.items():
            if k in GLOBAL_INPUTS:
                m[k] = v
            else:
                m[k + "_L0"] = v
        for k, v in m1.items():
            if k not in GLOBAL_INPUTS:
                m[k + "_L1"] = v
        ohs = np.zeros((128, 2), np.float32); ohs[:, s] = 1.0
        ohp = np.zeros((128, 2), np.float32); ohp[:, 1 - s] = 1.0
        m["oh_self"] = ohs; m["oh_part"] = ohp
        maps.append({k: m[k] for k in P.ins})
    res = run_bass_kernel_spmd(P.nc, maps, core_ids=list(range(8))).results
    out = np.empty((4, 4096, D), np.float32)
    for c in range(8):
        b, s = c // 2, c % 2
        yo = res[c]["outT"].T
        if s == 0:
            out[b, 0:2048] = yo
        else:
            out[b, 2048:4096] = yo[::-1]
    return out
```
